# Optimizing a Trainium2 kernel written in Bass

```python
import math
import jax, jax.numpy as jnp
from jax import lax
import numpy as np

D_MODEL = 1024
BATCH = 8
SEQ = 4096
DEPTH = 2

MEM_LEN = 256
NSA_HEADS = 8
NSA_GROUPS = 2
NSA_HPG = NSA_HEADS // NSA_GROUPS
NSA_DH = D_MODEL // 16
NSA_WIDTH = NSA_HEADS * NSA_DH
KV_WIDTH = NSA_GROUPS * NSA_DH
CMP_LEN = 32
CMP_STRIDE = 16
SEL_BLOCK = 64
SEL_TOP = 16
WINDOW = 512
NSA_QBLOCK = 64
CONV_WIDTH = D_MODEL // 2
CONV_K = 31
GLA_HEADS = 4
GLA_DK = D_MODEL // 16
GLA_DV = D_MODEL // 8
GLA_RANK = 16
GLA_TAU = 16.0
GLA_CHUNK = 64
REL_BUCKETS = 32
REL_MAX_DIST = 128
XA_HEADS = 4
XA_DH = D_MODEL // XA_HEADS
N_EXPERTS = 32
TOP_K = 4
D_EXPERT = D_MODEL
SWIGLU_ALPHA = 1.702
SWIGLU_LIMIT = 7.0
MOE_BLOCK = 128
N_BRANCH = 3
BRANCH_WIDTH = D_MODEL // 2
DEEPNORM_ALPHA = (2 * DEPTH) ** 0.25
DEEPNORM_BETA = (8 * DEPTH) ** -0.25
IN_SIZES = (NSA_WIDTH, 6 * KV_WIDTH, 3 * NSA_HEADS, 2 * CONV_WIDTH,
            GLA_HEADS * GLA_DK, GLA_HEADS * GLA_DK, GLA_HEADS * GLA_DV, GLA_RANK,
            GLA_HEADS * GLA_DV, N_BRANCH * D_MODEL)
D_IN = sum(IN_SIZES)
IN_OFFSETS = tuple(int(o) for o in np.cumsum(IN_SIZES)[:-1])

kernel_name = 'hybrid_nsa_conv_gla_moe_deepnorm'


def layer_norm(x, g, b, eps=1e-5):
    xf = x.astype(jnp.float32)
    mu = jnp.mean(xf, -1, keepdims=True)
    var = jnp.mean(jnp.square(xf - mu), -1, keepdims=True)
    return ((xf - mu) * lax.rsqrt(var + eps) * g + b).astype(x.dtype)


def rms_norm(x, g, eps=1e-6):
    xf = x.astype(jnp.float32)
    return (xf * lax.rsqrt(jnp.mean(jnp.square(xf), -1, keepdims=True) + eps) * g).astype(x.dtype)


def masked_softmax(s, mask):
    s = jnp.where(mask, s, -jnp.inf)
    m = jnp.max(s, -1, keepdims=True)
    m = jnp.where(jnp.isfinite(m), m, 0.0)
    p = jnp.exp(s - m)
    return p / jnp.maximum(jnp.sum(p, -1, keepdims=True), 1e-30)


def t5_bucket(dist):
    n = jnp.maximum(dist, 0)
    exact = REL_BUCKETS // 2
    log_ratio = jnp.log(jnp.maximum(n, 1).astype(jnp.float32) / exact) / math.log(REL_MAX_DIST / exact)
    large = exact + (log_ratio * (REL_BUCKETS - exact)).astype(jnp.int32)
    return jnp.where(n < exact, n, jnp.minimum(large, REL_BUCKETS - 1))


def compress_blocks(kv, pe, w1, b1, w2):
    S = kv.shape[1]
    n_cmp = (S - CMP_LEN) // CMP_STRIDE + 1
    idx = jnp.arange(n_cmp)[:, None] * CMP_STRIDE + jnp.arange(CMP_LEN)[None, :]
    blocks = kv[:, idx] + pe[None, None, :, None, :]
    h = jax.nn.gelu(jnp.einsum('bclgd,lde->bcge', blocks, w1) + b1)
    return h @ w2


def nsa_attention(q, k_cmp, v_cmp, k_sel, v_sel, k_win, v_win, gates, rel_bias):
    B, S, G, HG, DH = q.shape
    n_cmp = k_cmp.shape[1]
    n_slc = S // SEL_BLOCK
    n_sel = min(SEL_TOP, n_slc)
    n_tok = n_sel * SEL_BLOCK
    span = NSA_QBLOCK + WINDOW
    cmp_start = jnp.arange(n_cmp) * CMP_STRIDE
    cmp_end = cmp_start + CMP_LEN - 1
    blk = jnp.arange(n_slc)
    overlap = ((cmp_start[:, None] < (blk[None, :] + 1) * SEL_BLOCK)
               & (cmp_start[:, None] + CMP_LEN > blk[None, :] * SEL_BLOCK)).astype(jnp.float32)
    ks_blocks = k_sel.reshape(B, n_slc, SEL_BLOCK, G, DH).transpose(0, 3, 1, 2, 4)
    vs_blocks = v_sel.reshape(B, n_slc, SEL_BLOCK, G, DH).transpose(0, 3, 1, 2, 4)
    pad = ((0, 0), (WINDOW, 0), (0, 0), (0, 0))
    kw_pad = jnp.pad(k_win, pad)
    vw_pad = jnp.pad(v_win, pad)
    bias_hg = rel_bias.reshape(REL_BUCKETS, G, HG)
    gather_blocks = jax.vmap(jax.vmap(lambda blocks, ix: blocks[ix]))
    g_idx = jnp.arange(G)[None, :, None, None]

    def one_block(i):
        q0 = i * NSA_QBLOCK
        t = q0 + jnp.arange(NSA_QBLOCK)
        qb = lax.dynamic_slice_in_dim(q, q0, NSA_QBLOCK, axis=1)
        gb = lax.dynamic_slice_in_dim(gates, q0, NSA_QBLOCK, axis=1)
        dist_c = t[:, None] - cmp_end[None, :]
        s_c = (jnp.einsum('bqghd,bcgd->bqghc', qb, k_cmp).astype(jnp.float32)
               + bias_hg[t5_bucket(dist_c)].transpose(0, 2, 3, 1))
        p_c = masked_softmax(s_c, (dist_c >= 0)[:, None, None, :])
        o_c = jnp.einsum('bqghc,bcgd->bqghd', p_c.astype(v_cmp.dtype), v_cmp)
        cur = t // SEL_BLOCK
        imp = jnp.einsum('bqghc,cj->bqgj', p_c, overlap)
        future = blk[None, :] > cur[:, None]
        forced = (blk[None, :] == 0) | (blk[None, :] == cur[:, None]) | (blk[None, :] == cur[:, None] - 1)
        imp = jnp.where(forced[:, None, :], jnp.inf, jnp.where(future[:, None, :], -jnp.inf, imp))
        _, idx = lax.top_k(imp, n_sel)
        idx = idx.transpose(0, 2, 1, 3)
        k_g = gather_blocks(ks_blocks, idx).reshape(B, G, NSA_QBLOCK, n_tok, DH)
        v_g = gather_blocks(vs_blocks, idx).reshape(B, G, NSA_QBLOCK, n_tok, DH)
        pos = (idx[..., None] * SEL_BLOCK + jnp.arange(SEL_BLOCK)).reshape(B, G, NSA_QBLOCK, n_tok)
        dist_s = t[:, None] - pos
        s_s = (jnp.einsum('bqghd,bgqnd->bgqhn', qb, k_g).astype(jnp.float32)
               + bias_hg[t5_bucket(dist_s), g_idx].transpose(0, 1, 2, 4, 3))
        p_s = masked_softmax(s_s, (dist_s >= 0)[:, :, :, None, :])
        o_s = jnp.einsum('bgqhn,bgqnd->bqghd', p_s.astype(v_g.dtype), v_g)
        kw = lax.dynamic_slice_in_dim(kw_pad, q0, span, axis=1)
        vw = lax.dynamic_slice_in_dim(vw_pad, q0, span, axis=1)
        key_pos = q0 - WINDOW + jnp.arange(span)
        dist_w = t[:, None] - key_pos[None, :]
        mask_w = (dist_w >= 0) & (dist_w < WINDOW) & (key_pos[None, :] >= 0)
        s_w = (jnp.einsum('bqghd,blgd->bqghl', qb, kw).astype(jnp.float32)
               + bias_hg[t5_bucket(dist_w)].transpose(0, 2, 3, 1))
        p_w = masked_softmax(s_w, mask_w[:, None, None, :])
        o_w = jnp.einsum('bqghl,blgd->bqghd', p_w.astype(vw.dtype), vw)
        o = gb[..., 0:1] * o_c + gb[..., 1:2] * o_s + gb[..., 2:3] * o_w
        return o.reshape(B, NSA_QBLOCK, G * HG * DH)

    out = lax.map(one_block, jnp.arange(S // NSA_QBLOCK))
    return out.transpose(1, 0, 2, 3).reshape(B, S, G * HG * DH)


def conformer_conv(u_pair, w, b, g, beta):
    a, gate = jnp.split(u_pair, 2, axis=-1)
    u = a * jax.nn.sigmoid(gate)
    y = lax.conv_general_dilated(u, w[:, None, :], window_strides=(1,), padding=[(CONV_K - 1, 0)],
                                 dimension_numbers=('NWC', 'WIO', 'NWC'),
                                 feature_group_count=u.shape[-1]) + b
    return jax.nn.silu(layer_norm(y, g, beta))


def gla_chunked(q, k, v, log_a):
    B, S, H, DK = q.shape
    DV = v.shape[-1]
    C = GLA_CHUNK
    n = S // C

    def chunks(a):
        return a.astype(jnp.float32).reshape(B, n, C, H, a.shape[-1]).transpose(1, 0, 3, 2, 4)

    qc, kc, vc, ac = chunks(q), chunks(k), chunks(v), chunks(log_a)
    bc = jnp.cumsum(ac, axis=-2)
    causal = jnp.tril(jnp.ones((C, C), bool))[:, :, None]

    def step(state, inp):
        qi, ki, vi, bi = inp
        diff = bi[:, :, :, None, :] - bi[:, :, None, :, :]
        decay = jnp.exp(jnp.where(causal, diff, -jnp.inf))
        attn = jnp.einsum('bhtd,bhtsd->bhts', qi, decay * ki[:, :, None, :, :])
        o = attn @ vi + jnp.einsum('bhtd,bhde->bhte', qi * jnp.exp(bi), state)
        b_last = bi[:, :, -1:, :]
        state = (jnp.exp(b_last[:, :, 0, :, None]) * state
                 + jnp.einsum('bhsd,bhse->bhde', ki * jnp.exp(b_last - bi), vi))
        return state, o

    _, o = lax.scan(step, jnp.zeros((B, H, DK, DV), jnp.float32), (qc, kc, vc, bc))
    return o.transpose(1, 0, 3, 2, 4).reshape(B, S, H, DV)


def token_mixer(x, rel_bias, w_in, cmp_pe, cmp_w1, cmp_b1, cmp_w2, conv_w, conv_b, conv_g, conv_beta,
                gla_w, gla_b, gla_g, w_branch, w_out):
    B, S, D = x.shape
    z = x @ w_in
    q, kv, nsa_g, conv_in, gq, gk, gv, ga, gr, merge_g = jnp.split(z, IN_OFFSETS, axis=-1)
    q = q.reshape(B, S, NSA_GROUPS, NSA_HPG, NSA_DH) * NSA_DH ** -0.5
    kv = kv.reshape(B, S, 6, NSA_GROUPS, NSA_DH)
    k_cmp = compress_blocks(kv[:, :, 0], cmp_pe[0], cmp_w1[0], cmp_b1[0], cmp_w2[0])
    v_cmp = compress_blocks(kv[:, :, 1], cmp_pe[1], cmp_w1[1], cmp_b1[1], cmp_w2[1])
    nsa_gates = jax.nn.sigmoid(nsa_g).reshape(B, S, NSA_GROUPS, NSA_HPG, 3)
    out_a = nsa_attention(q, k_cmp, v_cmp, kv[:, :, 2], kv[:, :, 3], kv[:, :, 4], kv[:, :, 5],
                          nsa_gates, rel_bias)
    out_b = conformer_conv(conv_in, conv_w, conv_b, conv_g, conv_beta)
    log_a = jax.nn.log_sigmoid((ga @ gla_w + gla_b).astype(jnp.float32)) / GLA_TAU
    o = gla_chunked(gq.reshape(B, S, GLA_HEADS, GLA_DK) * GLA_DK ** -0.5,
                    gk.reshape(B, S, GLA_HEADS, GLA_DK),
                    gv.reshape(B, S, GLA_HEADS, GLA_DV),
                    log_a.reshape(B, S, GLA_HEADS, GLA_DK))
    out_c = rms_norm(o, gla_g).astype(x.dtype).reshape(B, S, GLA_HEADS * GLA_DV) * jax.nn.silu(gr)
    gates = jax.nn.sigmoid(merge_g).reshape(B, S, N_BRANCH, D)
    merged = (gates[:, :, 0] * (out_a @ w_branch[0])
              + gates[:, :, 1] * (out_b @ w_branch[1])
              + gates[:, :, 2] * (out_c @ w_branch[2]))
    return merged @ w_out


def cross_attention(x, mem, wq, wkv, wo):
    B, S, D = x.shape
    M = mem.shape[1]
    q = (x @ wq).reshape(B, S, XA_HEADS, XA_DH) * XA_DH ** -0.5
    kv = (mem @ wkv).reshape(B, M, 2, XA_HEADS, XA_DH)
    s = jnp.einsum('bshd,bmhd->bhsm', q, kv[:, :, 0]).astype(jnp.float32)
    p = jax.nn.softmax(s, axis=-1).astype(x.dtype)
    o = jnp.einsum('bhsm,bmhd->bshd', p, kv[:, :, 1]).reshape(B, S, D)
    return o @ wo


def moe_ffn(x, router_w, router_b, w_gu, b_gu, w_dn, b_dn):
    B, S, D = x.shape
    N = B * S
    A = N * TOP_K
    x2 = x.reshape(N, D)
    logits = (x2 @ router_w + router_b).astype(jnp.float32)
    top_v, top_i = lax.top_k(logits, TOP_K)
    gate = jax.nn.softmax(top_v, axis=-1).astype(x.dtype)
    e_flat = top_i.reshape(A)
    tok = jnp.arange(A, dtype=jnp.int32) // TOP_K
    g_flat = gate.reshape(A)
    order = jnp.argsort(e_flat)
    e_s, tok_s, g_s = e_flat[order], tok[order], g_flat[order]
    counts = jnp.bincount(e_flat, length=N_EXPERTS)
    starts = jnp.cumsum(counts) - counts
    padded = (counts + MOE_BLOCK - 1) // MOE_BLOCK * MOE_BLOCK
    pends = jnp.cumsum(padded)
    pstarts = pends - padded
    dest = pstarts[e_s] + jnp.arange(A, dtype=jnp.int32) - starts[e_s]
    n_slots = -(-(A + N_EXPERTS * MOE_BLOCK) // MOE_BLOCK) * MOE_BLOCK
    slot_tok = jnp.zeros((n_slots,), jnp.int32).at[dest].set(tok_s)
    slot_gate = jnp.zeros((n_slots,), x.dtype).at[dest].set(g_s)
    n_blocks = n_slots // MOE_BLOCK
    blk_e = jnp.minimum(jnp.searchsorted(pends, jnp.arange(n_blocks, dtype=jnp.int32) * MOE_BLOCK,
                                         side='right'), N_EXPERTS - 1)
    xs = x2[slot_tok].reshape(n_blocks, MOE_BLOCK, D)

    def expert_block(args):
        xb, gb, e = args
        gu = xb @ w_gu[e] + b_gu[e]
        g, u = gu[:, :D_EXPERT], gu[:, D_EXPERT:]
        g = jnp.minimum(g, SWIGLU_LIMIT)
        u = jnp.clip(u, -SWIGLU_LIMIT, SWIGLU_LIMIT)
        h = (u + 1.0) * g * jax.nn.sigmoid(SWIGLU_ALPHA * g)
        return (h @ w_dn[e] + b_dn[e]) * gb[:, None]

    ys = lax.map(expert_block, (xs, slot_gate.reshape(n_blocks, MOE_BLOCK), blk_e)).reshape(n_slots, D)
    return jax.ops.segment_sum(ys, slot_tok, num_segments=N).astype(x.dtype).reshape(B, S, D)


def setup_inputs(seed: int = 0) -> dict:
    key = jax.random.key(seed)
    keys = jax.random.split(key, 28)
    L, D = DEPTH, D_MODEL

    def nrm(i, shape, scale):
        return scale * jax.random.normal(keys[i], shape, jnp.float32)

    return {
        'x': nrm(0, (BATCH, SEQ, D), 1.0),
        'mem': nrm(1, (BATCH, MEM_LEN, D), 1.0),
        'rel_bias': nrm(2, (REL_BUCKETS, NSA_HEADS), 0.1),
        'w_in': nrm(3, (L, D, D_IN), D ** -0.5),
        'cmp_pe': nrm(4, (L, 2, CMP_LEN, NSA_DH), 0.1),
        'cmp_w1': nrm(5, (L, 2, CMP_LEN, NSA_DH, NSA_DH), (CMP_LEN * NSA_DH) ** -0.5),
        'cmp_b1': nrm(6, (L, 2, NSA_DH), 0.01),
        'cmp_w2': nrm(7, (L, 2, NSA_DH, NSA_DH), NSA_DH ** -0.5),
        'conv_w': nrm(8, (L, CONV_K, CONV_WIDTH), CONV_K ** -0.5),
        'conv_b': nrm(9, (L, CONV_WIDTH), 0.01),
        'conv_norm_g': 1.0 + nrm(10, (L, CONV_WIDTH), 0.01),
        'conv_norm_b': nrm(11, (L, CONV_WIDTH), 0.01),
        'gla_gate_w': nrm(12, (L, GLA_RANK, GLA_HEADS * GLA_DK), GLA_RANK ** -0.5),
        'gla_gate_b': nrm(13, (L, GLA_HEADS * GLA_DK), 0.1),
        'gla_norm_g': 1.0 + nrm(14, (L, GLA_DV), 0.01),
        'w_branch': nrm(15, (L, N_BRANCH, BRANCH_WIDTH, D), BRANCH_WIDTH ** -0.5 * DEEPNORM_BETA),
        'w_out': nrm(16, (L, D, D), D ** -0.5 * DEEPNORM_BETA),
        'xa_wq': nrm(17, (L, D, D), D ** -0.5),
        'xa_wkv': nrm(18, (L, D, 2 * D), D ** -0.5),
        'xa_wo': nrm(19, (L, D, D), D ** -0.5 * DEEPNORM_BETA),
        'router_w': nrm(20, (L, D, N_EXPERTS), D ** -0.5),
        'router_b': nrm(21, (L, N_EXPERTS), 0.01),
        'expert_w_gu': nrm(22, (L, N_EXPERTS, D, 2 * D_EXPERT), D ** -0.5),
        'expert_b_gu': nrm(23, (L, N_EXPERTS, 2 * D_EXPERT), 0.01),
        'expert_w_down': nrm(24, (L, N_EXPERTS, D_EXPERT, D), D_EXPERT ** -0.5 * DEEPNORM_BETA),
        'expert_b_down': nrm(25, (L, N_EXPERTS, D), 0.01),
        'norm_g': 1.0 + nrm(26, (L, 3, D), 0.01),
        'norm_b': nrm(27, (L, 3, D), 0.01),
    }


def reference(x, mem, rel_bias, w_in, cmp_pe, cmp_w1, cmp_b1, cmp_w2, conv_w, conv_b, conv_norm_g,
              conv_norm_b, gla_gate_w, gla_gate_b, gla_norm_g, w_branch, w_out, xa_wq, xa_wkv, xa_wo,
              router_w, router_b, expert_w_gu, expert_b_gu, expert_w_down, expert_b_down, norm_g, norm_b):
    for l in range(DEPTH):
        mix = token_mixer(x, rel_bias, w_in[l], cmp_pe[l], cmp_w1[l], cmp_b1[l], cmp_w2[l], conv_w[l],
                          conv_b[l], conv_norm_g[l], conv_norm_b[l], gla_gate_w[l], gla_gate_b[l],
                          gla_norm_g[l], w_branch[l], w_out[l])
        x = layer_norm(DEEPNORM_ALPHA * x + mix, norm_g[l, 0], norm_b[l, 0])
        xa = cross_attention(x, mem, xa_wq[l], xa_wkv[l], xa_wo[l])
        x = layer_norm(DEEPNORM_ALPHA * x + xa, norm_g[l, 1], norm_b[l, 1])
        ff = moe_ffn(x, router_w[l], router_b[l], expert_w_gu[l], expert_b_gu[l], expert_w_down[l],
                     expert_b_down[l])
        x = layer_norm(DEEPNORM_ALPHA * x + ff, norm_g[l, 2], norm_b[l, 2])
    return x
```

```python
import math
from contextlib import ExitStack

import numpy as np
import concourse.bass as bass
import concourse.mybir as mybir
from concourse.bass_utils import run_bass_kernel_spmd

F32 = mybir.dt.float32
BF16 = mybir.dt.bfloat16
I32 = mybir.dt.int32
AF = mybir.ActivationFunctionType
ALU = mybir.AluOpType
AX = mybir.AxisListType

D = 1024
S = 4096
NT = S // 128
DEPTH = 2
MEM = 256
D_IN = 6952
O_Q, O_KV, O_NG, O_CONV, O_GQ, O_GK, O_GV, O_GA, O_GR, O_MG = 0, 512, 1280, 1304, 2328, 2584, 2840, 3352, 3368, 3880
ALPHA = (2 * DEPTH) ** 0.25
NE = 32
CAP = 768
NEG = -30000.0


class Ev:
    __slots__ = ("key", "val", "snap")

    def __init__(self, key, val, snap):
        self.key, self.val, self.snap = key, val, snap


class Tk:
    __slots__ = ("w", "r", "name")

    def __init__(self, name=""):
        self.w = None
        self.r = {}
        self.name = name


class T:
    def __init__(self, h, name):
        self.h = h
        self.tk = Tk(name)

    def __getitem__(self, idx):
        return self.h[idx]


class KB:
    NDS = 8

    def __init__(self, nc):
        self.nc = nc
        self.es = ExitStack()
        self.E = {"pe": nc.tensor, "act": nc.scalar, "dve": nc.vector, "pool": nc.gpsimd, "sp": nc.sync}
        self.sem = {}
        self.cnt = {}
        self.known = {e: {} for e in self.E}
        for e in self.E:
            self.sem["c_" + e] = self.es.enter_context(nc.semaphore("c_" + e))
            self.cnt["c_" + e] = 0
        self.dring = {}
        for q in ("sp", "pool", "act"):
            ring = []
            for i in range(self.NDS):
                k = f"d_{q}{i}"
                self.sem[k] = self.es.enter_context(nc.semaphore(k))
                self.cnt[k] = 0
                ring.append(k)
            self.dring[q] = [ring, 0, {}]
        self.ninst = 0
        self.tiles = []

    def sb(self, name, shape, dt):
        self.uid = getattr(self, "uid", 0) + 1
        name = f"{name}_{self.uid}"
        t = T(self.es.enter_context(self.nc.sbuf_tensor(name, list(shape), dt)), name)
        self.tiles.append(t)
        return t

    def ps(self, name, shape, dt=F32):
        t = T(self.es.enter_context(self.nc.psum_tensor(name, list(shape), dt)), name)
        t.is_psum = True
        self.tiles.append(t)
        return t

    def _wait(self, e, ev):
        if ev is None:
            return
        kn = self.known[e]
        if kn.get(ev.key, 0) >= ev.val:
            return
        self.E[e].wait_ge(self.sem[ev.key], ev.val)
        self.ninst += 1
        for k, v in ev.snap.items():
            if kn.get(k, 0) < v:
                kn[k] = v
        kn[ev.key] = ev.val

    def _deps(self, e, outs, ins):
        for t in ins:
            tk = t.tk if isinstance(t, T) else t
            self._wait(e, tk.w)
        for t in outs:
            tk = t.tk if isinstance(t, T) else t
            self._wait(e, tk.w)
            for r in list(tk.r.values()):
                self._wait(e, r)

    def _record(self, ev, outs, ins):
        for t in ins:
            tk = t.tk if isinstance(t, T) else t
            tk.r[ev.key] = ev
        for t in outs:
            tk = t.tk if isinstance(t, T) else t
            tk.w = ev
            tk.r = {}

    def op(self, e, outs, ins, fn, n=1):
        outs = list(outs) + [t for t in ins if getattr(t, "is_psum", False) and t not in outs]
        self._deps(e, outs, ins)
        inst = fn()
        key = "c_" + e
        self.cnt[key] += 1
        inst.then_inc(self.sem[key], 1)
        self.ninst += n
        ev = Ev(key, self.cnt[key], dict(self.known[e]))
        self._record(ev, outs, ins)
        return ev

    def dma(self, q, out, in_, outs, ins, **kw):
        e = q
        ring, idx, last = self.dring[q]
        key = ring[idx % self.NDS]
        self.dring[q][1] = idx + 1
        self._deps(e, outs, ins)
        self._wait(e, last.get(key))
        inst = self.E[e].dma_start(out=out, in_=in_, **kw)
        self.cnt[key] += 16
        inst.then_inc(self.sem[key], 16)
        self.ninst += 1
        ev = Ev(key, self.cnt[key], dict(self.known[e]))
        last[key] = ev
        self._record(ev, outs, ins)
        return ev

    def idma(self, out, out_off, in_, in_off, outs, ins, **kw):
        e = "pool"
        ring, idx, last = self.dring[e]
        key = ring[idx % self.NDS]
        self.dring[e][1] = idx + 1
        self._deps(e, outs, ins)
        self._wait(e, last.get(key))
        inst = self.nc.gpsimd.indirect_dma_start(out=out, out_offset=out_off, in_=in_, in_offset=in_off, **kw)
        self.cnt[key] += 16
        inst.then_inc(self.sem[key], 16)
        self.ninst += 1
        ev = Ev(key, self.cnt[key], dict(self.known[e]))
        last[key] = ev
        self._record(ev, outs, ins)
        return ev

    def barrier(self, extra=()):
        evs = []
        for e in self.E:
            key = "c_" + e
            if self.cnt[key]:
                evs.append(Ev(key, self.cnt[key], {}))
        for q in self.dring:
            for key, ev in self.dring[q][2].items():
                evs.append(Ev(key, self.cnt[key], {}))
        for e in self.E:
            for ev in evs:
                self._wait(e, ev)
        for t in self.tiles:
            t.tk.w = None
            t.tk.r = {}
        for tk in extra:
            tk.w = None
            tk.r = {}

    def push(self):
        self._saved = getattr(self, "_saved", [])
        self._saved.append((self.es, self.tiles))
        self.es = ExitStack()
        self.tiles = list(self.tiles)

    def pop(self):
        self.barrier()
        self.es.close()
        self.es, self.tiles = self._saved.pop()


WNAMES = ["rel_bias", "w_in", "cmp_pe", "cmp_w1", "cmp_b1", "cmp_w2", "conv_w", "conv_b", "conv_norm_g",
          "conv_norm_b", "gla_gate_w", "gla_gate_b", "gla_norm_g", "w_branch", "w_out", "xa_wq", "xa_wkv", "xa_wo",
          "router_w", "router_b", "expert_w_gu", "expert_b_gu", "expert_w_down", "expert_b_down", "norm_g", "norm_b"]
WSHAPES = {
    "rel_bias": (32, 8), "w_in": (2, 1024, 6952), "cmp_pe": (2, 2, 32, 64), "cmp_w1": (2, 2, 32, 64, 64),
    "cmp_b1": (2, 2, 64), "cmp_w2": (2, 2, 64, 64), "conv_w": (2, 31, 512), "conv_b": (2, 512),
    "conv_norm_g": (2, 512), "conv_norm_b": (2, 512), "gla_gate_w": (2, 16, 256), "gla_gate_b": (2, 256),
    "gla_norm_g": (2, 128), "w_branch": (2, 3, 512, 1024), "w_out": (2, 1024, 1024), "xa_wq": (2, 1024, 1024),
    "xa_wkv": (2, 1024, 2048), "xa_wo": (2, 1024, 1024), "router_w": (2, 1024, 32), "router_b": (2, 32),
    "expert_w_gu": (2, 32, 1024, 2048), "expert_b_gu": (2, 32, 2048), "expert_w_down": (2, 32, 1024, 1024),
    "expert_b_down": (2, 32, 1024), "norm_g": (2, 3, 1024), "norm_b": (2, 3, 1024),
}


class Prog:
    def __init__(self, dbg=(), use=None):
        self.dbg = set(dbg)
        nc = self.nc = bass.Bass("TRN2", target_bir_lowering=False)
        self.kb = KB(nc)
        self.din = {}
        self.din["x"] = nc.dram_tensor("x", [S, D], F32, kind="ExternalInput").ap()
        self.din["mem"] = nc.dram_tensor("mem", [MEM, D], F32, kind="ExternalInput").ap()
        for n in WNAMES:
            if use is None or n in use:
                self.din[n] = nc.dram_tensor(n, list(WSHAPES[n]), F32, kind="ExternalInput").ap()
        self.din["c_ident"] = nc.dram_tensor("c_ident", [128, 128], F32, kind="ExternalInput").ap()
        self.din["c_tab"] = nc.dram_tensor("c_tab", [128, C_TAB_W], F32, kind="ExternalInput").ap()
        self.din["c_esel"] = nc.dram_tensor("c_esel", [64, 32 * 128], F32, kind="ExternalInput").ap()
        self.y = nc.dram_tensor("y", [S, D], F32, kind="ExternalOutput").ap()
        self.scr = {}

    def dram(self, name, shape, dt):
        kind = "ExternalOutput" if name in self.dbg else "Internal"
        ap = self.nc.dram_tensor(name, list(shape), dt, kind=kind).ap()
        self.scr[name] = ap
        return ap

    def consts(self):
        kb, nc = self.kb, self.nc
        self.ident = kb.sb("ident", [128, 128], F32)
        kb.dma("sp", self.ident[:], self.din["c_ident"][:, :], [self.ident], [])
        self.identb = kb.sb("identb", [128, 128], BF16)
        kb.op("dve", [self.identb], [self.ident], lambda: nc.vector.tensor_copy(out=self.identb[:], in_=self.ident[:]))
        self.onesb = kb.sb("onesb", [128, 128], BF16)
        kb.op("dve", [self.onesb], [], lambda: nc.vector.memset(self.onesb[:], 1.0))
        self.onesf = kb.sb("onesf", [128, 128], F32)
        kb.op("dve", [self.onesf], [], lambda: nc.vector.memset(self.onesf[:], 1.0))
        self.eps5 = kb.sb("eps5", [128, 1], F32)
        kb.op("dve", [self.eps5], [], lambda: nc.vector.memset(self.eps5[:], 1e-5))
        self.eps6 = kb.sb("eps6", [128, 1], F32)
        kb.op("dve", [self.eps6], [], lambda: nc.vector.memset(self.eps6[:], 1e-6))
        self.psb = [kb.ps(f"psb{i}", [128, 512], F32) for i in range(7)]
        self.ps_bf = kb.ps("ps_bf", [128, 1024], BF16)
        self.psi = 0
        self.nrot = 7

    def bank(self):
        b = self.psb[self.psi % self.nrot]
        self.psi += 1
        return b

    def build_xT(self, src, xT, ntiles=NT, dt_out=BF16, stg=None):
        kb, nc = self.kb, self.nc
        if stg is None:
            stg = [kb.sb(f"xstg{i}", [128, D], F32) for i in range(2)]
        for i in range(ntiles):
            st = stg[i % 2]
            kb.dma("sp", st[:], src[i * 128:(i + 1) * 128, :], [st], [])
            for half in range(2):
                pb = self.bank()
                def f(pb=pb, st=st, half=half):
                    for j in range(4):
                        kc = half * 4 + j
                        ins = nc.tensor.transpose(out=pb[:, j * 128:(j + 1) * 128], in_=st[:, kc * 128:(kc + 1) * 128], identity=self.ident[:])
                    return ins
                kb.op("pe", [pb], [st, self.ident], f, n=4)
                eng = "act" if half == 0 else "dve"
                dst = xT[:, half * 4:half * 4 + 4, i * 128:(i + 1) * 128]
                srcp = pb[:].rearrange("p (j t) -> p j t", j=4)
                if eng == "act":
                    kb.op("act", [xT], [pb], lambda dst=dst, srcp=srcp: nc.scalar.copy(out=dst, in_=srcp))
                else:
                    kb.op("dve", [xT], [pb], lambda dst=dst, srcp=srcp: nc.vector.tensor_copy(out=dst, in_=srcp))

    def p1_inproj(self, l, xsrc, parts=("fm", "tm"), ngrp_lim=None):
        kb, nc = self.kb, self.nc
        zT = self.scr["zT"]
        w_in = self.din["w_in"]
        kb.push()
        xT = kb.sb("xT", [128, 8, S], BF16)
        self.build_xT(xsrc, xT)
        wv = w_in[l].rearrange("(kc p) n -> p kc n", p=128)
        wb = [kb.sb(f"p1w{i}", [128, 8, 512], BF16) for i in range(2)]
        zst = [kb.sb(f"p1z{i}", [128, S], F32) for i in range(2)]
        ngrp = (D_IN + 511) // 512
        if ngrp_lim:
            ngrp = ngrp_lim
        ci = 0
        for g in range(ngrp if "fm" in parts else 0):
            c0 = g * 512
            ncol = min(512, D_IN - c0)
            w = wb[g % 2]
            kb.dma("pool", w[:, :, 0:ncol], wv[:, :, c0:c0 + ncol], [w], [])
            for cc in range(0, ncol, 128):
                m = min(128, ncol - cc)
                z = zst[ci % 2]
                ci += 1
                for tg in range(8):
                    pb = self.bank()
                    def f(pb=pb, w=w, cc=cc, m=m, tg=tg):
                        for kc in range(8):
                            ins = nc.tensor.matmul(pb[0:m, :], w[:, kc, cc:cc + m], xT[:, kc, tg * 512:(tg + 1) * 512],
                                                   start=(kc == 0), stop=(kc == 7))
                        return ins
                    kb.op("pe", [pb], [w, xT], f, n=8)
                    if tg % 2 == 0:
                        kb.op("act", [z], [pb], lambda pb=pb, z=z, m=m, tg=tg: nc.scalar.copy(out=z[0:m, tg * 512:(tg + 1) * 512], in_=pb[0:m, :]))
                    else:
                        kb.op("dve", [z], [pb], lambda pb=pb, z=z, m=m, tg=tg: nc.vector.tensor_copy(out=z[0:m, tg * 512:(tg + 1) * 512], in_=pb[0:m, :]))
                kb.dma("sp", zT[c0 + cc:c0 + cc + m, :], z[0:m, :], [], [z])
        wt = kb.sb("p1wt", [128, 8, 792], BF16)
        for (o, n, d0) in ((896, 128, 0), (1152, 152, 128), (O_GV, 512, 280)):
            kb.dma("pool", wt[:, :, d0:d0 + n], wv[:, :, o:o + n], [wt], [])
        vst = kb.sb("p1v", [128, NT, 256], BF16)
        gst = kb.sb("p1g", [128, NT, 24], F32)
        gvst = kb.sb("p1gv", [128, NT, 512], BF16)
        for i in range(NT if "tm" in parts else 0):
            pa = self.bank()
            pg = self.bank()
            def fa(pa=pa, i=i):
                for kc in range(8):
                    ins = nc.tensor.matmul(pa[:, 0:280], xT[:, kc, i * 128:(i + 1) * 128], wt[:, kc, 0:280], start=(kc == 0), stop=(kc == 7))
                return ins
            kb.op("pe", [pa], [wt, xT], fa, n=8)
            def fg(pg=pg, i=i):
                for kc in range(8):
                    ins = nc.tensor.matmul(pg[:, :], xT[:, kc, i * 128:(i + 1) * 128], wt[:, kc, 280:792], start=(kc == 0), stop=(kc == 7))
                return ins
            kb.op("pe", [pg], [wt, xT], fg, n=8)
            kb.op("dve", [vst], [pa], lambda pa=pa, i=i: nc.vector.tensor_copy(out=vst[:, i, :], in_=pa[:, 0:256]))
            kb.op("act", [gst], [pa], lambda pa=pa, i=i: nc.scalar.activation(out=gst[:, i, :], in_=pa[:, 256:280], func=AF.Sigmoid))
            kb.op("act", [gvst], [pg], lambda pg=pg, i=i: nc.scalar.copy(out=gvst[:, i, :], in_=pg[:, :]))
        kb.dma("sp", self.scr["vtm"][:, :], vst[:].rearrange("p i c -> p (i c)"), [], [vst])
        kb.dma("sp", self.scr["ngate"][:, :], gst[:].rearrange("p i c -> p (i c)"), [], [gst])
        kb.dma("sp", self.scr["gvtm"][:, :], gvst[:].rearrange("p i c -> p (i c)"), [], [gvst])
        kb.pop()

    def alloc_scratch(self):
        self.dram("zT", [D_IN, S], F32)
        self.dram("vtm", [128, NT * 256], BF16)
        self.dram("ngate", [128, NT * 24], F32)
        self.dram("gvtm", [128, NT * 512], BF16)
        for i in range(3):
            self.dram(f"brT{i}", [512, S], BF16)
        for nm in ("x1", "x2", "x3"):
            self.dram(nm, [S, D], F32)
        self.dram("moe_xs", [NE * CAP, D], BF16)
        self.dram("moe_ys", [NE * CAP, D], F32)
        if "kcT" in self.dbg:
            self.dram("kcT", [64, 512], BF16)
            self.dram("vc", [128, 260], BF16)

    def finish(self):
        self.kb.barrier()

    def bias_setup(self):
        kb, nc = self.kb, self.nc
        rb = kb.sb("rb_rep", [128, 32, 8], F32)
        src = self.din["rel_bias"].rearrange("(o b) h -> o (b h)", o=1).to_broadcast([128, 256])
        kb.dma("sp", rb[:].rearrange("p b h -> p (b h)"), src, [rb], [])
        self.ndelta = kb.sb("ndelta", [128, 32, 8], F32)
        kb.op("dve", [self.ndelta], [rb], lambda: nc.vector.tensor_tensor(out=self.ndelta[:, 1:32, :], in0=rb[:, 0:31, :], in1=rb[:, 1:32, :], op=ALU.subtract))

    def make_bias_table(self, dist_t, dist, n, outs, tmp):
        kb, nc = self.kb, self.nc
        dt_, m0, mk = tmp
        kb.op("dve", [m0], [dist_t], lambda: nc.vector.tensor_scalar(out=m0[:, 0:n], in0=dist[:, 0:n], scalar1=0.0, scalar2=NEG, op0=ALU.is_lt, op1=ALU.mult))
        for h in range(8):
            o_t, o_ap = outs[h]
            kb.op("pool", [o_t], [m0], lambda o_ap=o_ap: nc.gpsimd.tensor_copy(out=o_ap, in_=m0[:, 0:n]))
        for b in range(1, 32):
            thr = float(T5_THR[b])
            kb.op("dve", [mk], [dist_t], lambda thr=thr: nc.vector.tensor_scalar(out=mk[:, 0:n], in0=dist[:, 0:n], scalar1=thr, scalar2=None, op0=ALU.is_lt))
            for h in range(8):
                o_t, o_ap = outs[h]
                eng = "dve" if h % 2 == 0 else "dve"
                kb.op(eng, [o_t], [mk, self.ndelta], lambda o_ap=o_ap, b=b, h=h: nc.vector.scalar_tensor_tensor(
                    out=o_ap, in0=mk[:, 0:n], scalar=self.ndelta[:, b, h:h + 1], in1=o_ap, op0=ALU.mult, op1=ALU.add))


def _t5_thresholds():
    n = np.arange(0, 4096, dtype=np.int32)
    exact = 16
    lr = np.log(np.maximum(n, 1).astype(np.float32) / np.float32(exact)) / np.float32(math.log(128 / exact))
    large = exact + (lr * np.float32(32 - exact)).astype(np.int32)
    bucket = np.where(n < exact, n, np.minimum(large, 31))
    thr = np.zeros(32, np.int64)
    for b in range(1, 32):
        thr[b] = int(np.min(n[bucket >= b]))
    return thr, bucket


T5_THR, T5_BUCKET = _t5_thresholds()


def _const_tab():
    q = np.arange(128, dtype=np.float32)[:, None]
    u = np.arange(504, dtype=np.float32)[None, :]
    dist_c = q - 16.0 * (u - 248.0) - 31.0
    k = np.arange(128, dtype=np.float32)[:, None]
    qq = np.arange(128, dtype=np.float32)[None, :]
    dist0 = qq - k
    dist1 = 128.0 + qq - k
    m4 = np.where(qq < k, 0.0, NEG).astype(np.float32)
    rel = np.arange(-62, 64)[None, :]
    ql = np.arange(128)[:, None]
    cur_off = (ql >= 64).astype(np.int64)
    d = rel - cur_off
    ftab = np.where(d > 0, -1.0e4, np.where(d == 0, 2.0e4, np.where(d == -1, 1.0e4, 0.0))).astype(np.float32)
    tab = np.concatenate([dist_c, dist0, dist1, m4, ftab], axis=1).astype(np.float32)
    return np.ascontiguousarray(tab)


def _const_esel():
    e = np.zeros((64, 32, 128), np.float32)
    for kt in range(32):
        e[2 * kt, kt, 0:64] = 1.0
        e[2 * kt + 1, kt, 64:128] = 1.0
    return e.reshape(64, 32 * 128)


C_TAB_W = 504 + 128 * 3 + 126


def _nsa(self, l):
    kb, nc = self.kb, self.nc
    zT = self.scr["zT"]
    kb.push()
    tab = kb.sb("ctab", [128, C_TAB_W], F32)
    kb.dma("sp", tab[:], self.din["c_tab"][:, :], [tab], [])
    esel = kb.sb("esel", [64, 32, 128], BF16)
    kb.dma("pool", esel[:].rearrange("p a b -> p (a b)"), self.din["c_esel"][:, :], [esel], [])
    Wc = [kb.sb(f"Wc{g}", [128, 4, 504], F32) for g in range(2)]
    BT0 = [kb.sb(f"BT0{g}", [128, 4, 128], F32) for g in range(2)]
    BT1 = [kb.sb(f"BT1{g}", [128, 4, 128], F32) for g in range(2)]
    tmp = (None, kb.sb("btm0", [128, 504], F32), kb.sb("btmk", [128, 504], F32))
    self.make_bias_table(tab, tab[:, 0:504], 504, [(Wc[h // 4], Wc[h // 4][:, h % 4, :]) for h in range(8)], tmp)
    self.make_bias_table(tab, tab[:, 504:632], 128, [(BT0[h // 4], BT0[h // 4][:, h % 4, :]) for h in range(8)], tmp)
    self.make_bias_table(tab, tab[:, 632:760], 128, [(BT1[h // 4], BT1[h // 4][:, h % 4, :]) for h in range(8)], tmp)
    BT4 = kb.sb("BT4", [128, 4, 128], F32)
    for hg in range(4):
        kb.op("pool", [BT4], [tab], lambda hg=hg: nc.gpsimd.tensor_copy(out=BT4[:, hg, :], in_=tab[:, 760:888]))
    kcT = kb.sb("kcT_sb", [64, 2, 256], BF16)
    vc = kb.sb("vc_sb", [128, 2, 2, 65], BF16)
    kb.op("dve", [kcT], [], lambda: nc.vector.memset(kcT[:], 0.0))
    kb.op("dve", [vc], [], lambda: nc.vector.memset(vc[:], 0.0))
    kb.op("dve", [vc], [], lambda: nc.vector.memset(vc[:, :, :, 64:65], 1.0))
    kv = [kb.sb(f"cmpkv{i}", [64, S], BF16) for i in range(2)]
    w1a = kb.sb("cmpw1", [64, 32, 64], BF16)
    pe_sb = kb.sb("cmppe", [32, 64], F32)
    peT = kb.sb("cmppeT", [64, 32], BF16)
    b1 = kb.sb("cmpb1", [64, 1], F32)
    cb = kb.sb("cmpcb", [64, 1], F32)
    w2 = kb.sb("cmpw2", [64, 64], BF16)
    xs = kb.sb("cmpxs", [64, 256], F32)
    x2 = kb.sb("cmpx2", [64, 256], F32)
    sg = kb.sb("cmpsg", [64, 256], F32)
    hT = kb.sb("cmphT", [64, 256], BF16)
    kb.op("dve", [hT], [], lambda: nc.vector.memset(hT[:], 0.0))
    for i in range(2):
        kb.dma("pool", w1a[:], self.din["cmp_w1"][l, i].rearrange("l d e -> d l e"), [w1a], [])
        kb.dma("sp", pe_sb[:], self.din["cmp_pe"][l, i], [pe_sb], [])
        kb.dma("sp", b1[:], self.din["cmp_b1"][l, i].rearrange("(e o) -> e o", o=1), [b1], [])
        kb.dma("pool", w2[:], self.din["cmp_w2"][l, i], [w2], [])
        pb = self.bank()
        kb.op("pe", [pb], [pe_sb, self.ident], lambda pb=pb: nc.tensor.transpose(out=pb[0:64, 0:32], in_=pe_sb[:, :], identity=self.ident[0:32, 0:32]))
        kb.op("dve", [peT], [pb], lambda pb=pb: nc.vector.tensor_copy(out=peT[:], in_=pb[0:64, 0:32]))
        pb = self.bank()
        def fc0(pb=pb):
            for ll in range(32):
                ins = nc.tensor.matmul(pb[0:64, 0:1], w1a[:, ll, :], peT[:, ll:ll + 1], start=(ll == 0), stop=(ll == 31))
            return ins
        kb.op("pe", [pb], [w1a, peT], fc0, n=32)
        kb.op("dve", [cb], [pb, b1], lambda pb=pb: nc.vector.tensor_tensor(out=cb[:], in0=pb[0:64, 0:1], in1=b1[:], op=ALU.add))
        for g in range(2):
            kvt = kv[g]
            r0 = O_KV + i * 128 + g * 64
            kb.dma("pool", kvt[:], zT[r0:r0 + 64, :], [kvt], [])
            kvr = kvt[:].rearrange("p (c s) -> p c s", s=16)
            pb = self.bank()
            def facc(pb=pb, kvr=kvr):
                for ll in range(32):
                    c0, s_ = (0, ll) if ll < 16 else (1, ll - 16)
                    ins = nc.tensor.matmul(pb[0:64, 0:255], w1a[:, ll, :], kvr[:, c0:c0 + 255, s_], start=(ll == 0), stop=(ll == 31))
                return ins
            kb.op("pe", [pb], [w1a, kvt], facc, n=32)
            kb.op("act", [xs], [pb, cb], lambda pb=pb: nc.scalar.activation(out=xs[:, 0:255], in_=pb[0:64, 0:255], func=AF.Identity, bias=cb[:, 0:1], scale=1.0))
            kb.op("dve", [x2], [xs], lambda: nc.vector.tensor_tensor(out=x2[:, 0:255], in0=xs[:, 0:255], in1=xs[:, 0:255], op=ALU.mult))
            kb.op("dve", [x2], [x2], lambda: nc.vector.tensor_scalar(out=x2[:, 0:255], in0=x2[:, 0:255], scalar1=0.044715, scalar2=1.0, op0=ALU.mult, op1=ALU.add))
            kb.op("dve", [x2], [x2, xs], lambda: nc.vector.tensor_tensor(out=x2[:, 0:255], in0=x2[:, 0:255], in1=xs[:, 0:255], op=ALU.mult))
            kb.op("act", [sg], [x2], lambda: nc.scalar.activation(out=sg[:, 0:255], in_=x2[:, 0:255], func=AF.Sigmoid, scale=1.5957691216057308))
            kb.op("dve", [hT], [sg, xs], lambda: nc.vector.tensor_tensor(out=hT[:, 0:255], in0=sg[:, 0:255], in1=xs[:, 0:255], op=ALU.mult))
            if i == 0:
                pb = self.bank()
                kb.op("pe", [pb], [w2, hT], lambda pb=pb: nc.tensor.matmul(pb[0:64, 0:255], w2[:, :], hT[:, 0:255], start=True, stop=True))
                kb.op("act", [kcT], [pb], lambda pb=pb, g=g: nc.scalar.copy(out=kcT[:, g, 0:255], in_=pb[0:64, 0:255]))
            else:
                for ct in range(2):
                    m = 128 if ct == 0 else 127
                    pb = self.bank()
                    kb.op("pe", [pb], [w2, hT], lambda pb=pb, ct=ct, m=m: nc.tensor.matmul(pb[0:m, 0:64], hT[:, ct * 128:ct * 128 + m], w2[:, :], start=True, stop=True))
                    kb.op("act", [vc], [pb], lambda pb=pb, ct=ct, m=m, g=g: nc.scalar.copy(out=vc[0:m, ct, g, 0:64], in_=pb[0:m, 0:64]))
    if "kcT" in self.dbg:
        kb.dma("sp", self.scr["kcT"][:, :], kcT[:].rearrange("p a b -> p (a b)"), [], [kcT])
        kb.dma("sp", self.scr["vc"][:, :], vc[:].rearrange("p a b c -> p (a b c)"), [], [vc])
    self._nsa_main(l, dict(tab=tab, esel=esel, Wc=Wc, BT0=BT0, BT1=BT1, BT4=BT4, kcT=kcT, vc=vc))
    kb.pop()


Prog.p23_nsa = _nsa


def _nsa_main(self, l, R):
    kb, nc = self.kb, self.nc
    zT = self.scr["zT"]
    tab, esel, kcT, vc = R["tab"], R["esel"], R["kcT"], R["vc"]
    vstg = kb.sb("n_vstg", [128, NT, 4, 64], BF16)
    kb.dma("sp", vstg[:].rearrange("p i a d -> p (i a d)"), self.scr["vtm"][:, :], [vstg], [])
    gt = kb.sb("n_gt", [128, NT, 24], F32)
    kb.dma("sp", gt[:].rearrange("p i c -> p (i c)"), self.scr["ngate"][:, :], [gt], [])
    q4 = kb.sb("n_q4", [64, 4, S], BF16)
    ks = kb.sb("n_ks", [64, S], BF16)
    kw = kb.sb("n_kw", [64, S], BF16)
    va = kb.sb("n_va", [128, NT, 2, 65], BF16)
    sc = kb.sb("n_sc", [128, 4, 256], F32)
    pc = kb.sb("n_pc", [128, 4, 256], F32)
    mx = kb.sb("n_mx", [128, 4], F32)
    sm = kb.sb("n_sm", [128, 4], F32)
    rs = kb.sb("n_rs", [128, 4], F32)
    pacc = kb.sb("n_pacc", [128, 64, 4], F32)
    imp = kb.sb("n_imp", [128, 64], F32)
    imp2 = kb.sb("n_imp2", [128, 64], F32)
    m8a = kb.sb("n_m8a", [128, 8], F32)
    m8b = kb.sb("n_m8b", [128, 8], F32)
    selm = kb.sb("n_selm", [128, 64], F32)
    mrT = kb.sb("n_mrT", [64, 4, 128], BF16)
    pcT = kb.sb("n_pcT", [128, 4, 2, 128], BF16)
    coef = kb.sb("n_coef", [128, 3, 4], F32)
    oacc = kb.sb("n_oacc", [128, 4, 64], F32)
    otmp = kb.sb("n_otmp", [128, 4, 64], F32)
    oaT = kb.sb("n_oaT", [128, 2, 128], BF16)
    sTf = [kb.sb(f"n_sTf{i}", [128, 512], F32) for i in range(2)]
    pT = [kb.sb(f"n_pT{i}", [128, 512], BF16) for i in range(3)]
    pacc_f = pacc[:].rearrange("p j r -> p (j r)")
    brT0 = self.scr["brT0"].rearrange("(kc p) t -> p kc t", p=128)
    cnt = {"s": 0, "p": 0}
    pv_win, pv_sel = self.psb[5], self.psb[6]
    self.nrot = 5

    def exp_tile(pb, bias_t, eng_alt):
        p_t = pT[cnt["p"] % 3]
        cnt["p"] += 1
        if bias_t is not None:
            s_t = sTf[cnt["s"] % 2]
            cnt["s"] += 1
            kb.op("dve", [s_t], [pb, bias_t], lambda: nc.vector.scalar_tensor_tensor(
                out=s_t[:], in0=pb[:, :], scalar=0.125, in1=bias_t[:].rearrange("p a b -> p (a b)"), op0=ALU.mult, op1=ALU.add))
            kb.op("act", [p_t], [s_t], lambda: nc.scalar.activation(out=p_t[:], in_=s_t[:], func=AF.Exp))
        else:
            kb.op("act", [p_t], [pb], lambda: nc.scalar.activation(out=p_t[:], in_=pb[:, :], func=AF.Exp, scale=0.125))
        return p_t

    def finish_branch(pvb, br, jt, g, first):
        pv3 = pvb[:, 0:260].rearrange("p (h d) -> p h d", d=65)
        cf = coef[:, br, :]
        kb.op("dve", [coef], [pvb], lambda: nc.vector.tensor_scalar(out=cf, in0=pv3[:, :, 64], scalar1=1e-30, scalar2=None, op0=ALU.max))
        kb.op("dve", [coef], [coef], lambda: nc.vector.reciprocal(out=cf, in_=cf))
        gsl = gt[:, jt, g * 12:(g + 1) * 12].rearrange("p (h b) -> p h b", b=3)[:, :, br]
        kb.op("dve", [coef], [coef, gt], lambda: nc.vector.tensor_tensor(out=cf, in0=cf, in1=gsl, op=ALU.mult))
        cfb = cf.to_broadcast([128, 4, 64]) if False else coef[:, br, :, None].to_broadcast([128, 4, 64])
        if first:
            kb.op("dve", [oacc], [pvb, coef], lambda: nc.vector.tensor_tensor(out=oacc[:], in0=pv3[:, :, 0:64], in1=cfb, op=ALU.mult))
        else:
            kb.op("dve", [otmp], [pvb, coef], lambda: nc.vector.tensor_tensor(out=otmp[:], in0=pv3[:, :, 0:64], in1=cfb, op=ALU.mult))
            kb.op("pool", [oacc], [otmp, oacc], lambda: nc.gpsimd.tensor_tensor(out=oacc[:], in0=oacc[:], in1=otmp[:], op=ALU.add))

    for g in range(2):
        Wc, BT0, BT1, BT4 = R["Wc"][g], R["BT0"][g], R["BT1"][g], R["BT4"]
        kb.dma("pool", q4[:], zT[g * 256:(g + 1) * 256, :].rearrange("(h d) t -> d h t", d=64), [q4], [])
        kb.dma("pool", ks[:], zT[O_KV + 256 + g * 64:O_KV + 256 + g * 64 + 64, :], [ks], [])
        kb.dma("pool", kw[:], zT[O_KV + 512 + g * 64:O_KV + 512 + g * 64 + 64, :], [kw], [])
        kb.op("dve", [va], [], lambda: nc.vector.memset(va[:], 1.0))
        kb.op("pool", [va], [vstg], lambda g=g: nc.gpsimd.tensor_copy(out=va[:, :, 0, 0:64], in_=vstg[:, :, g, :]))
        kb.op("pool", [va], [vstg], lambda g=g: nc.gpsimd.tensor_copy(out=va[:, :, 1, 0:64], in_=vstg[:, :, 2 + g, :]))
        for jt in range(NT):
            qs = slice(jt * 128, (jt + 1) * 128)
            for half in range(2):
                pb = self.bank()
                def fsc(pb=pb, half=half):
                    for j in range(2):
                        ins = nc.tensor.matmul(pb[:, j * 256:(j + 1) * 256], q4[:, half * 2 + j, qs], kcT[:, g, :], start=True, stop=True)
                    return ins
                kb.op("pe", [pb], [q4, kcT], fsc, n=2)
                kb.op("dve", [sc], [pb, Wc], lambda pb=pb, half=half: nc.vector.scalar_tensor_tensor(
                    out=sc[:, half * 2:half * 2 + 2, :], in0=pb[:, :].rearrange("p (a b) -> p a b", a=2), scalar=0.125,
                    in1=Wc[:, half * 2:half * 2 + 2, 248 - 8 * jt:248 - 8 * jt + 256], op0=ALU.mult, op1=ALU.add))
            kb.op("dve", [mx], [sc], lambda: nc.vector.tensor_reduce(out=mx[:], in_=sc[:], axis=AX.X, op=ALU.max))
            kb.op("dve", [mx], [mx], lambda: nc.vector.tensor_scalar(out=mx[:], in0=mx[:], scalar1=-20000.0, scalar2=-1.0, op0=ALU.max, op1=ALU.mult))
            kb.op("dve", [sm], [], lambda: nc.vector.memset(sm[:], 0.0))
            for hg in range(4):
                kb.op("act", [pc, sm], [sc, mx], lambda hg=hg: nc.scalar.activation(out=pc[:, hg, :], in_=sc[:, hg, :], func=AF.Exp,
                                                                               bias=mx[:, hg:hg + 1], scale=1.0, accum_out=sm[:, hg:hg + 1]))
            kb.op("dve", [rs], [sm], lambda: nc.vector.tensor_scalar(out=rs[:], in0=sm[:], scalar1=1e-30, scalar2=None, op0=ALU.max))
            kb.op("dve", [rs], [rs], lambda: nc.vector.reciprocal(out=rs[:], in_=rs[:]))
            kb.op("dve", [pacc], [pc, rs], lambda: nc.vector.tensor_scalar(out=pacc_f, in0=pc[:, 0, :], scalar1=rs[:, 0:1], scalar2=None, op0=ALU.mult))
            for hg in range(1, 4):
                kb.op("dve", [pacc], [pc, rs, pacc], lambda hg=hg: nc.vector.scalar_tensor_tensor(
                    out=pacc_f, in0=pc[:, hg, :], scalar=rs[:, hg:hg + 1], in1=pacc_f, op0=ALU.mult, op1=ALU.add))
            kb.op("dve", [imp], [pacc], lambda: nc.vector.tensor_reduce(out=imp[:], in_=pacc[:], axis=AX.X, op=ALU.add))
            kb.op("dve", [imp], [imp, pacc], lambda: nc.vector.tensor_tensor(out=imp[:, 1:64], in0=imp[:, 1:64], in1=pacc[:, 0:63, 3], op=ALU.add))
            kb.op("dve", [imp], [imp, tab], lambda: nc.vector.tensor_tensor(out=imp[:], in0=imp[:], in1=tab[:, 888 + 62 - 2 * jt:888 + 62 - 2 * jt + 64], op=ALU.add))
            kb.op("dve", [imp], [imp], lambda: nc.vector.memset(imp[:, 0:1], 3.0e4))
            kb.op("dve", [m8a], [imp], lambda: nc.vector.max(out=m8a[:], in_=imp[:]))
            kb.op("dve", [imp2], [m8a, imp], lambda: nc.vector.match_replace(out=imp2[:], in_to_replace=m8a[:], in_values=imp[:], imm_value=-1.0e9))
            kb.op("dve", [m8b], [imp2], lambda: nc.vector.max(out=m8b[:], in_=imp2[:]))
            kb.op("dve", [selm], [imp, m8b], lambda: nc.vector.tensor_scalar(out=selm[:], in0=imp[:], scalar1=m8b[:, 7:8], scalar2=-240000.0, op0=ALU.is_lt, op1=ALU.mult))
            pb = self.bank()
            kb.op("pe", [pb], [selm, self.ident], lambda pb=pb: nc.tensor.transpose(out=pb[0:64, 0:128], in_=selm[:, :], identity=self.ident[:, :]))
            for hg in range(4):
                if hg % 2 == 0:
                    kb.op("act", [mrT], [pb], lambda pb=pb, hg=hg: nc.scalar.copy(out=mrT[:, hg, :], in_=pb[0:64, 0:128]))
                else:
                    kb.op("dve", [mrT], [pb], lambda pb=pb, hg=hg: nc.vector.tensor_copy(out=mrT[:, hg, :], in_=pb[0:64, 0:128]))
            nct = 2 if jt >= 16 else 1
            for half in range(2):
                pb = self.bank()
                def ftr(pb=pb, half=half):
                    for j in range(2):
                        for ct in range(nct):
                            ins = nc.tensor.transpose(out=pb[:, (j * 2 + ct) * 128:(j * 2 + ct + 1) * 128], in_=pc[:, half * 2 + j, ct * 128:(ct + 1) * 128], identity=self.ident[:, :])
                    return ins
                kb.op("pe", [pb], [pc, self.ident], ftr, n=2 * nct)
                src = pb[:, :].rearrange("p (j c q) -> p j c q", j=2, c=2)[:, :, 0:nct, :]
                kb.op("act" if half == 0 else "dve", [pcT], [pb],
                      (lambda src=src, half=half: nc.scalar.copy(out=pcT[:, half * 2:half * 2 + 2, 0:nct, :], in_=src)) if half == 0 else
                      (lambda src=src, half=half: nc.vector.tensor_copy(out=pcT[:, half * 2:half * 2 + 2, 0:nct, :], in_=src)))
            pvc = self.bank()
            def fpvc(pvc=pvc):
                for hg in range(4):
                    for ct in range(nct):
                        ins = nc.tensor.matmul(pvc[:, hg * 65:(hg + 1) * 65], pcT[:, hg, ct, :], vc[:, ct, g, :], start=(ct == 0 and hg == 0), stop=(ct == nct - 1 and hg == 3))
                return ins
            kb.op("pe", [pvc], [pcT, vc], fpvc, n=4 * nct)
            finish_branch(pvc, 0, jt, g, True)
            kts = [kt for kt in range(jt - 4, jt + 1) if kt >= 0]
            for ii, kt in enumerate(kts):
                dl = jt - kt
                pb = self.bank()
                kb.op("pe", [pb], [kw, q4], lambda pb=pb, kt=kt: nc.tensor.matmul(pb[:, :], kw[:, kt * 128:(kt + 1) * 128], q4[:, :, qs], start=True, stop=True))
                p_t = exp_tile(pb, {0: BT0, 1: BT1, 4: BT4}.get(dl), ii)
                def fpv(p_t=p_t, kt=kt, ii=ii):
                    for hg in range(4):
                        ins = nc.tensor.matmul(pv_win[:, hg * 65:(hg + 1) * 65], p_t[:, hg * 128:(hg + 1) * 128], va[:, kt, 1, :], start=(ii == 0 and hg == 0), stop=(ii == len(kts) - 1 and hg == 3))
                    return ins
                kb.op("pe", [pv_win], [p_t, va], fpv, n=4)
            finish_branch(pv_win, 2, jt, g, False)
            for kt in range(jt + 1):
                dl = jt - kt
                pb = self.bank()
                def fss(pb=pb, kt=kt):
                    nc.tensor.matmul(pb[:, :], ks[:, kt * 128:(kt + 1) * 128], q4[:, :, qs], start=True, stop=False)
                    return nc.tensor.matmul(pb[:, :], esel[:, kt, :], mrT[:, :, :], start=False, stop=True)
                kb.op("pe", [pb], [ks, q4, esel, mrT], fss, n=2)
                p_t = exp_tile(pb, {0: BT0, 1: BT1}.get(dl), kt)
                def fpv2(p_t=p_t, kt=kt):
                    for hg in range(4):
                        ins = nc.tensor.matmul(pv_sel[:, hg * 65:(hg + 1) * 65], p_t[:, hg * 128:(hg + 1) * 128], va[:, kt, 0, :], start=(kt == 0 and hg == 0), stop=(kt == jt and hg == 3))
                    return ins
                kb.op("pe", [pv_sel], [p_t, va], fpv2, n=4)
            finish_branch(pv_sel, 1, jt, g, False)
            pb = self.bank()
            oflat = oacc[:].rearrange("p h d -> p (h d)")
            def fto(pb=pb):
                for j in range(2):
                    ins = nc.tensor.transpose(out=pb[:, j * 128:(j + 1) * 128], in_=oflat[:, j * 128:(j + 1) * 128], identity=self.ident[:, :])
                return ins
            kb.op("pe", [pb], [oacc, self.ident], fto, n=2)
            kb.op("act", [oaT], [pb], lambda pb=pb: nc.scalar.copy(out=oaT[:], in_=pb[:, 0:256].rearrange("p (j q) -> p j q", j=2)))
            kb.dma("sp", brT0[:, g * 2:g * 2 + 2, qs], oaT[:], [], [oaT])
    self.nrot = 7


Prog._nsa_main = _nsa_main


def _conv(self, l):
    kb, nc = self.kb, self.nc
    zT = self.scr["zT"]
    brT1 = self.scr["brT1"]
    kb.push()
    prm = kb.sb("cv_prm", [34, 512], F32)
    kb.dma("sp", prm[0:31, :], self.din["conv_w"][l], [prm], [])
    kb.dma("sp", prm[31:32, :], self.din["conv_b"][l:l + 1, :], [prm], [])
    kb.dma("sp", prm[32:33, :], self.din["conv_norm_g"][l:l + 1, :], [prm], [])
    kb.dma("sp", prm[33:34, :], self.din["conv_norm_b"][l:l + 1, :], [prm], [])
    cw = kb.sb("cv_w", [128, 4, 34], F32)
    pb = self.bank()
    def ft(pb=pb):
        for cc in range(4):
            ins = nc.tensor.transpose(out=pb[:, cc * 34:(cc + 1) * 34], in_=prm[:, cc * 128:(cc + 1) * 128], identity=self.ident[0:34, 0:34])
        return ins
    kb.op("pe", [pb], [prm, self.ident], ft, n=4)
    kb.op("dve", [cw], [pb], lambda: nc.vector.tensor_copy(out=cw[:], in_=pb[:, 0:136].rearrange("p (c k) -> p c k", c=4)))
    y4 = kb.sb("cv_y", [128, 4, S], F32)
    a_t = kb.sb("cv_a", [128, S], F32)
    g_t = kb.sb("cv_g", [128, S], F32)
    u = kb.sb("cv_u", [128, 30 + S], F32)
    kb.op("dve", [u], [], lambda: nc.vector.memset(u[:, 0:30], 0.0))
    for cc in range(4):
        kb.dma("sp", a_t[:], zT[O_CONV + cc * 128:O_CONV + (cc + 1) * 128, :], [a_t], [])
        kb.dma("sp", g_t[:], zT[O_CONV + 512 + cc * 128:O_CONV + 512 + (cc + 1) * 128, :], [g_t], [])
        kb.op("act", [g_t], [g_t], lambda: nc.scalar.activation(out=g_t[:], in_=g_t[:], func=AF.Sigmoid))
        kb.op("pool", [u], [a_t, g_t], lambda: nc.gpsimd.tensor_tensor(out=u[:, 30:30 + S], in0=a_t[:], in1=g_t[:], op=ALU.mult))
        kb.op("dve", [y4], [u, cw], lambda cc=cc: nc.vector.tensor_scalar(out=y4[:, cc, :], in0=u[:, 0:S], scalar1=cw[:, cc, 0:1], scalar2=cw[:, cc, 31:32], op0=ALU.mult, op1=ALU.add))
        for k in range(1, 31):
            kb.op("dve", [y4], [u, cw, y4], lambda cc=cc, k=k: nc.vector.scalar_tensor_tensor(
                out=y4[:, cc, :], in0=u[:, k:k + S], scalar=cw[:, cc, k:k + 1], in1=y4[:, cc, :], op0=ALU.mult, op1=ALU.add))
    sq = kb.sb("cv_sq", [128, 4, 512], F32)
    mean = kb.sb("cv_mean", [128, 512], F32)
    msq = kb.sb("cv_msq", [128, 512], F32)
    rstd = kb.sb("cv_rstd", [128, 512], F32)
    tt = kb.sb("cv_tt", [128, 512], F32)
    ob = [kb.sb(f"cv_ob{i}", [128, 4, 512], BF16) for i in range(2)]
    brv = brT1.rearrange("(kc p) t -> p kc t", p=128)
    for tg in range(8):
        ts_ = slice(tg * 512, (tg + 1) * 512)
        kb.op("act", [sq], [y4], lambda: nc.scalar.activation(out=sq[:], in_=y4[:, :, ts_], func=AF.Square))
        p1, p2 = self.bank(), self.bank()
        def fs(p1=p1):
            for cc in range(4):
                ins = nc.tensor.matmul(p1[:, :], self.onesf[:, :], y4[:, cc, ts_], start=(cc == 0), stop=(cc == 3))
            return ins
        kb.op("pe", [p1], [y4, self.onesf], fs, n=4)
        def fq(p2=p2):
            for cc in range(4):
                ins = nc.tensor.matmul(p2[:, :], self.onesf[:, :], sq[:, cc, :], start=(cc == 0), stop=(cc == 3))
            return ins
        kb.op("pe", [p2], [sq, self.onesf], fq, n=4)
        kb.op("act", [mean], [p1], lambda p1=p1: nc.scalar.activation(out=mean[:], in_=p1[:, :], func=AF.Copy, scale=1.0 / 512))
        kb.op("dve", [msq], [mean], lambda: nc.vector.tensor_tensor(out=msq[:], in0=mean[:], in1=mean[:], op=ALU.mult))
        kb.op("dve", [rstd], [p2, msq], lambda p2=p2: nc.vector.scalar_tensor_tensor(out=rstd[:], in0=p2[:, :], scalar=1.0 / 512, in1=msq[:], op0=ALU.mult, op1=ALU.subtract))
        kb.op("act", [rstd], [rstd], lambda: nc.scalar.activation(out=rstd[:], in_=rstd[:], func=AF.Sqrt, bias=self.eps5[:, 0:1], scale=1.0))
        kb.op("dve", [rstd], [rstd], lambda: nc.vector.reciprocal(out=rstd[:], in_=rstd[:]))
        o_t = ob[tg % 2]
        for cc in range(4):
            kb.op("pool", [tt], [y4, mean], lambda cc=cc: nc.gpsimd.tensor_tensor(out=tt[:], in0=y4[:, cc, ts_], in1=mean[:], op=ALU.subtract))
            kb.op("dve", [tt], [tt, rstd], lambda: nc.vector.tensor_tensor(out=tt[:], in0=tt[:], in1=rstd[:], op=ALU.mult))
            kb.op("act", [o_t], [tt, cw], lambda cc=cc, o_t=o_t: nc.scalar.activation(out=o_t[:, cc, :], in_=tt[:], func=AF.Silu, scale=cw[:, cc, 32:33], bias=cw[:, cc, 33:34]))
        kb.dma("sp", brv[:, :, ts_], o_t[:], [], [o_t])
    kb.pop()


Prog.p4_conv = _conv


def _gla(self, l):
    kb, nc = self.kb, self.nc
    zT = self.scr["zT"]
    kb.push()
    qT = kb.sb("gl_qT", [64, 4, S], BF16)
    kT = kb.sb("gl_kT", [64, 4, S], BF16)
    ebl = kb.sb("gl_ebl", [64, 4, NT], F32)
    gw = kb.sb("gl_gw", [16, 256], F32)
    kb.dma("sp", gw[:], self.din["gla_gate_w"][l], [gw], [])
    gb = kb.sb("gl_gb", [64, 4], F32)
    kb.dma("sp", gb[:], self.din["gla_gate_b"][l].rearrange("(h d) -> d h", d=64), [gb], [], allow_slow_non_contiguous=True)
    kb.op("dve", [gb], [gb], lambda: nc.vector.tensor_scalar(out=gb[:], in0=gb[:], scalar1=-1.0, scalar2=None, op0=ALU.mult))
    gng = kb.sb("gl_gng", [128, 1], F32)
    kb.dma("sp", gng[:], self.din["gla_norm_g"][l].rearrange("(p o) -> p o", o=1), [gng], [])
    kb.push()
    ga = kb.sb("gl_ga", [16, S], F32)
    kb.dma("sp", ga[:], zT[O_GA:O_GA + 16, :], [ga], [])
    rmask = kb.sb("gl_rm", [64, S], F32)
    kb.op("pool", [rmask], [], lambda: nc.gpsimd.memset(rmask[:], 1.0))
    kb.op("pool", [rmask], [], lambda: nc.gpsimd.memset(rmask[:].rearrange("p (n c) -> p n c", c=128)[:, :, 0:1], 0.0))
    la = kb.sb("gl_la", [64, S], F32)
    cs = kb.sb("gl_cs", [64, S], F32)
    eb = kb.sb("gl_eb", [64, S], F32)
    st = kb.sb("gl_st", [64, S], F32)
    one1 = kb.sb("gl_one", [64, 1], F32)
    kb.op("dve", [one1], [], lambda: nc.vector.memset(one1[:], 1.0))
    for h in range(4):
        for tg in range(8):
            ts_ = slice(tg * 512, (tg + 1) * 512)
            pb = self.bank()
            kb.op("pe", [pb], [gw, ga], lambda pb=pb, ts_=ts_: nc.tensor.matmul(pb[0:64, :], gw[:, h * 64:(h + 1) * 64], ga[:, ts_], start=True, stop=True))
            kb.op("act", [la], [pb, gb], lambda pb=pb, ts_=ts_: nc.scalar.activation(out=la[:, ts_], in_=pb[0:64, :], func=AF.Exp, scale=-1.0, bias=gb[:, h:h + 1]))
        kb.op("act", [la], [la, one1], lambda: nc.scalar.activation(out=la[:], in_=la[:], func=AF.Ln, bias=one1[:, 0:1], scale=1.0))
        kb.op("dve", [cs], [la, rmask], lambda: nc.vector.tensor_tensor_scan(out=cs[:], data0=rmask[:], data1=la[:], initial=0.0, op0=ALU.mult, op1=ALU.add))
        kb.op("act", [eb], [cs], lambda: nc.scalar.activation(out=eb[:], in_=cs[:], func=AF.Exp, scale=-1.0 / 16))
        kb.op("pool", [ebl], [eb], lambda: nc.gpsimd.tensor_copy(out=ebl[:, h, :], in_=eb[:].rearrange("p (n c) -> p n c", c=128)[:, :, 127]))
        kb.dma("sp", st[:], zT[O_GQ + h * 64:O_GQ + (h + 1) * 64, :], [st], [])
        kb.op("dve", [qT], [st, eb], lambda: nc.vector.scalar_tensor_tensor(out=qT[:, h, :], in0=st[:], scalar=0.125, in1=eb[:], op0=ALU.mult, op1=ALU.mult))
        kb.op("act", [eb], [cs], lambda: nc.scalar.activation(out=eb[:], in_=cs[:], func=AF.Exp, scale=1.0 / 16))
        kb.dma("sp", st[:], zT[O_GK + h * 64:O_GK + (h + 1) * 64, :], [st], [])
        kb.op("dve", [kT], [st, eb], lambda: nc.vector.tensor_tensor(out=kT[:, h, :], in0=st[:], in1=eb[:], op=ALU.mult))
    kb.pop()
    v = kb.sb("gl_v", [128, NT, 512], BF16)
    kb.dma("sp", v[:].rearrange("p i c -> p (i c)"), self.scr["gvtm"][:, :], [v], [])
    sgr = kb.sb("gl_sgr", [128, 4, S], BF16)
    stg = kb.sb("gl_stg", [128, S], F32)
    for c in range(4):
        kb.dma("sp", stg[:], zT[O_GR + c * 128:O_GR + (c + 1) * 128, :], [stg], [])
        kb.op("act", [sgr], [stg], lambda c=c: nc.scalar.activation(out=sgr[:, c, :], in_=stg[:], func=AF.Silu))
    ocT = kb.sb("gl_ocT", [128, 4, S], BF16)
    cmask = kb.sb("gl_cmask", [128, 4, 128], F32)
    kb.op("pool", [cmask], [], lambda: nc.gpsimd.memset(cmask[:], 1.0))
    for h in range(4):
        kb.op("pool", [cmask], [cmask], lambda h=h: nc.gpsimd.affine_select(out=cmask[:, h, :], in_=cmask[:, h, :], pattern=[[1, 128]], compare_op=ALU.is_ge, fill=0.0, base=0, channel_multiplier=-1))
    S4 = kb.sb("gl_S4", [64, 4, 128], F32)
    S4b = [kb.sb(f"gl_S4b{i}", [64, 4, 128], BF16) for i in range(2)]
    tmpS = kb.sb("gl_tmpS", [64, 4, 128], F32)
    kb.op("dve", [S4], [], lambda: nc.vector.memset(S4[:], 0.0))
    kb.op("dve", [S4b[0]], [], lambda: nc.vector.memset(S4b[0][:], 0.0))
    attn = [kb.sb(f"gl_attn{i}", [128, 4, 128], BF16) for i in range(2)]
    ktm = [kb.sb(f"gl_ktm{i}", [128, 4, 64], BF16) for i in range(2)]
    sqo = kb.sb("gl_sqo", [128, 512], F32)
    rinv = kb.sb("gl_rinv", [128, 512], F32)
    on = kb.sb("gl_on", [128, 512], F32)
    for i in range(NT):
        tsl = slice(i * 128, (i + 1) * 128)
        Sb = S4b[i % 2]
        Sb_next = S4b[(i + 1) % 2]
        at = attn[i % 2]
        kt_ = ktm[i % 2]
        pb = self.bank()
        def fsc(pb=pb):
            for h in range(4):
                ins = nc.tensor.matmul(pb[:, h * 128:(h + 1) * 128], kT[:, h, tsl], qT[:, h, tsl], start=True, stop=True)
            return ins
        kb.op("pe", [pb], [kT, qT], fsc, n=4)
        kb.op("dve", [at], [pb, cmask], lambda pb=pb, at=at: nc.vector.tensor_tensor(out=at[:].rearrange("p a b -> p (a b)"), in0=pb[:, :], in1=cmask[:].rearrange("p a b -> p (a b)"), op=ALU.mult))
        pt = self.ps_bf
        def ftr(pt=pt):
            for h in range(4):
                ins = nc.tensor.transpose(out=pt[:, h * 64:(h + 1) * 64], in_=kT[:, h, tsl], identity=self.identb[0:64, 0:64])
            return ins
        kb.op("pe", [pt], [kT, self.identb], ftr, n=4)
        kb.op("act", [kt_], [pt], lambda pt=pt, kt_=kt_: nc.scalar.copy(out=kt_[:].rearrange("p a b -> p (a b)"), in_=pt[:, 0:256]))
        po = self.bank()
        def fo(po=po, at=at, Sb=Sb):
            for h in range(4):
                nc.tensor.matmul(po[:, h * 128:(h + 1) * 128], v[:, i, h * 128:(h + 1) * 128], at[:, h, :], start=(h == 0), stop=False)
            for h in range(4):
                ins = nc.tensor.matmul(po[:, h * 128:(h + 1) * 128], Sb[:, h, :], qT[:, h, tsl], start=False, stop=(h == 3))
            return ins
        kb.op("pe", [po], [v, at, Sb, qT], fo, n=8)
        pd = self.bank()
        def fd(pd=pd, kt_=kt_):
            for h in range(4):
                ins = nc.tensor.matmul(pd[0:64, h * 128:(h + 1) * 128], kt_[:, h, :], v[:, i, h * 128:(h + 1) * 128], start=(h == 0), stop=(h == 3))
            return ins
        kb.op("pe", [pd], [kt_, v], fd, n=4)
        kb.op("dve", [tmpS], [pd, S4], lambda pd=pd: nc.vector.tensor_tensor(out=tmpS[:], in0=pd[0:64, :].rearrange("p (h e) -> p h e", h=4), in1=S4[:], op=ALU.add))
        kb.op("dve", [S4], [tmpS, ebl], lambda: nc.vector.tensor_tensor(out=S4[:], in0=tmpS[:], in1=ebl[:, :, i:i + 1].to_broadcast([64, 4, 128]), op=ALU.mult))
        kb.op("act", [Sb_next], [S4], lambda Sb_next=Sb_next: nc.scalar.copy(out=Sb_next[:], in_=S4[:]))
        kb.op("act", [sqo], [po], lambda po=po: nc.scalar.activation(out=sqo[:], in_=po[:, :], func=AF.Square))
        pq = self.bank()
        kb.op("pe", [pq], [sqo, self.onesf], lambda pq=pq: nc.tensor.matmul(pq[:, :], self.onesf[:, :], sqo[:], start=True, stop=True))
        kb.op("act", [rinv], [pq], lambda pq=pq: nc.scalar.activation(out=rinv[:], in_=pq[:, :], func=AF.Sqrt, bias=self.eps6[:, 0:1], scale=1.0 / 128))
        kb.op("dve", [rinv], [rinv], lambda: nc.vector.reciprocal(out=rinv[:], in_=rinv[:]))
        kb.op("dve", [on], [po, rinv], lambda po=po: nc.vector.tensor_tensor(out=on[:], in0=po[:, :], in1=rinv[:], op=ALU.mult))
        kb.op("dve", [ocT], [on, gng, sgr], lambda: nc.vector.scalar_tensor_tensor(out=ocT[:, :, tsl], in0=on[:].rearrange("p (h t) -> p h t", h=4), scalar=gng[:, 0:1], in1=sgr[:, :, tsl], op0=ALU.mult, op1=ALU.mult))
    kb.dma("sp", self.scr["brT2"].rearrange("(kc p) t -> p kc t", p=128), ocT[:], [], [ocT])
    kb.pop()


Prog.p5_gla = _gla


def _ln_setup(self, l, idx):
    kb = self.kb
    lng = kb.sb("ln_g", [128, D], F32)
    lnb = kb.sb("ln_b", [128, D], F32)
    kb.dma("sp", lng[:], self.din["norm_g"][l, idx:idx + 1, :].to_broadcast([128, D]), [lng], [])
    kb.dma("sp", lnb[:], self.din["norm_b"][l, idx:idx + 1, :].to_broadcast([128, D]), [lnb], [])
    st = kb.sb("ln_st", [128, 2, 6], F32)
    mv = kb.sb("ln_mv", [128, 2], F32)
    return dict(g=lng, b=lnb, st=st, mv=mv)


def _ln_tile(self, L, h, o):
    kb, nc = self.kb, self.nc
    st, mv = L["st"], L["mv"]
    for j in range(2):
        kb.op("dve", [st], [h], lambda j=j: nc.vector.bn_stats(out=st[:, j, :], in_=h[:, j * 512:(j + 1) * 512]))
    kb.op("dve", [mv], [st], lambda: nc.vector.bn_aggr(out=mv[:], in_=st[:].rearrange("p a b -> p (a b)")))
    kb.op("act", [mv], [mv], lambda: nc.scalar.activation(out=mv[:, 1:2], in_=mv[:, 1:2], func=AF.Sqrt, bias=self.eps5[:, 0:1], scale=1.0))
    kb.op("dve", [mv], [mv], lambda: nc.vector.reciprocal(out=mv[:, 1:2], in_=mv[:, 1:2]))
    kb.op("dve", [h], [h, mv], lambda: nc.vector.tensor_scalar(out=h[:], in0=h[:], scalar1=mv[:, 0:1], scalar2=mv[:, 1:2], op0=ALU.subtract, op1=ALU.mult))
    kb.op("pool", [h], [h, L["g"]], lambda: nc.gpsimd.tensor_tensor(out=h[:], in0=h[:], in1=L["g"][:], op=ALU.mult))
    kb.op("pool", [o], [h, L["b"]], lambda: nc.gpsimd.tensor_tensor(out=o[:], in0=h[:], in1=L["b"][:], op=ALU.add))


Prog.ln_setup = _ln_setup
Prog.ln_tile = _ln_tile


def _proj_res_ln(self, L, aT, w, xsrc, xdst, tg, xin, hbuf, obuf, cnt):
    kb, nc = self.kb, self.nc
    for tt_ in range(4):
        row0 = tg * 512 + tt_ * 128
        x_t = xin[cnt[0] % 2]
        h_t = hbuf[cnt[0] % 2]
        o_t = obuf[cnt[0] % 2]
        cnt[0] += 1
        kb.dma("sp", x_t[:], xsrc[row0:row0 + 128, :], [x_t], [])
        for half in range(2):
            pb = self.bank()
            def f(pb=pb, half=half, tt_=tt_):
                for kc in range(8):
                    ins = nc.tensor.matmul(pb[:, :], aT[:, kc, tt_ * 128:(tt_ + 1) * 128], w[:, kc, half * 512:(half + 1) * 512], start=(kc == 0), stop=(kc == 7))
                return ins
            kb.op("pe", [pb], [aT, w], f, n=8)
            kb.op("dve", [h_t], [x_t, pb], lambda pb=pb, half=half, x_t=x_t, h_t=h_t: nc.vector.scalar_tensor_tensor(
                out=h_t[:, half * 512:(half + 1) * 512], in0=x_t[:, half * 512:(half + 1) * 512], scalar=ALPHA, in1=pb[:, :], op0=ALU.mult, op1=ALU.add))
        self.ln_tile(L, h_t, o_t)
        kb.dma("sp", xdst[row0:row0 + 128, :], o_t[:], [], [o_t])


Prog.proj_res_ln = _proj_res_ln


def _merge(self, l, xsrc, xdst):
    kb, nc = self.kb, self.nc
    zT = self.scr["zT"]
    kb.push()
    wbr = kb.sb("mg_wbr", [128, 3, 4, D], BF16)
    for br in range(3):
        kb.dma("pool", wbr[:, br, :, :], self.din["w_branch"][l, br].rearrange("(kc p) n -> p kc n", p=128), [wbr], [])
    wo = kb.sb("mg_wo", [128, 8, D], BF16)
    kb.dma("pool", wo[:], self.din["w_out"][l].rearrange("(kc p) n -> p kc n", p=128), [wo], [])
    L = self.ln_setup(l, 0)
    brs = kb.sb("mg_br", [128, 3, 4, 512], BF16)
    mg = kb.sb("mg_g", [128, 24, 512], F32)
    mT = kb.sb("mg_mT", [128, 8, 512], BF16)
    t1 = kb.sb("mg_t1", [128, 512], F32)
    t2 = kb.sb("mg_t2", [128, 512], F32)
    xin = [kb.sb(f"mg_x{i}", [128, D], F32) for i in range(2)]
    hb = [kb.sb(f"mg_h{i}", [128, D], F32) for i in range(2)]
    ob = [kb.sb(f"mg_o{i}", [128, D], F32) for i in range(2)]
    cnt = [0]
    mgv = zT[O_MG:O_MG + 3072, :].rearrange("(c p) t -> p c t", p=128)
    for tg in range(8):
        ts_ = slice(tg * 512, (tg + 1) * 512)
        for br in range(3):
            kb.dma("sp", brs[:, br, :, :], self.scr[f"brT{br}"].rearrange("(kc p) t -> p kc t", p=128)[:, :, ts_], [brs], [])
        for c3 in range(3):
            kb.dma("act", mg[:, c3 * 8:(c3 + 1) * 8, :], mgv[:, c3 * 8:(c3 + 1) * 8, ts_], [mg], [])
        kb.op("act", [mg], [mg], lambda: nc.scalar.activation(out=mg[:], in_=mg[:], func=AF.Sigmoid))
        for dm in range(8):
            pbs = []
            for br in range(3):
                pb = self.bank()
                def f(pb=pb, br=br, dm=dm):
                    for kc in range(4):
                        ins = nc.tensor.matmul(pb[:, :], wbr[:, br, kc, dm * 128:(dm + 1) * 128], brs[:, br, kc, :], start=(kc == 0), stop=(kc == 3))
                    return ins
                kb.op("pe", [pb], [wbr, brs], f, n=4)
                pbs.append(pb)
            kb.op("dve", [t1], [pbs[0], mg], lambda pbs=pbs, dm=dm: nc.vector.tensor_tensor(out=t1[:], in0=pbs[0][:, :], in1=mg[:, dm, :], op=ALU.mult))
            kb.op("dve", [t2], [pbs[1], mg], lambda pbs=pbs, dm=dm: nc.vector.tensor_tensor(out=t2[:], in0=pbs[1][:, :], in1=mg[:, 8 + dm, :], op=ALU.mult))
            kb.op("pool", [t1], [t1, t2], lambda: nc.gpsimd.tensor_tensor(out=t1[:], in0=t1[:], in1=t2[:], op=ALU.add))
            kb.op("dve", [t2], [pbs[2], mg], lambda pbs=pbs, dm=dm: nc.vector.tensor_tensor(out=t2[:], in0=pbs[2][:, :], in1=mg[:, 16 + dm, :], op=ALU.mult))
            kb.op("pool", [mT], [t1, t2], lambda dm=dm: nc.gpsimd.tensor_tensor(out=mT[:, dm, :], in0=t1[:], in1=t2[:], op=ALU.add))
        self.proj_res_ln(L, mT, wo, xsrc, xdst, tg, xin, hb, ob, cnt)
    kb.pop()


Prog.p6_merge = _merge


def _xattn(self, l, xsrc, xdst):
    kb, nc = self.kb, self.nc
    kb.push()
    wq = kb.sb("xa_wq", [128, 8, D], BF16)
    kb.dma("pool", wq[:], self.din["xa_wq"][l].rearrange("(kc p) n -> p kc n", p=128), [wq], [])
    wo = kb.sb("xa_wo", [128, 8, D], BF16)
    kb.dma("pool", wo[:], self.din["xa_wo"][l].rearrange("(kc p) n -> p kc n", p=128), [wo], [])
    L = self.ln_setup(l, 1)
    memT = kb.sb("xa_memT", [128, 8, MEM], BF16)
    self.build_xT(self.din["mem"], memT, ntiles=2)
    kTm = kb.sb("xa_kT", [128, 8, MEM], BF16)
    vm = kb.sb("xa_v", [128, 2, D], BF16)
    wkv = [kb.sb(f"xa_wkv{i}", [128, 8, 512], BF16) for i in range(2)]
    wkvv = self.din["xa_wkv"][l].rearrange("(kc p) n -> p kc n", p=128)
    for grp in range(4):
        w = wkv[grp % 2]
        kb.dma("pool", w[:], wkvv[:, :, grp * 512:(grp + 1) * 512], [w], [])
        if grp < 2:
            for cc in range(4):
                pb = self.bank()
                def f(pb=pb, w=w, cc=cc):
                    for kc in range(8):
                        ins = nc.tensor.matmul(pb[:, 0:MEM], w[:, kc, cc * 128:(cc + 1) * 128], memT[:, kc, :], start=(kc == 0), stop=(kc == 7))
                    return ins
                kb.op("pe", [pb], [w, memT], f, n=8)
                kb.op("act", [kTm], [pb], lambda pb=pb, j=grp * 4 + cc: nc.scalar.copy(out=kTm[:, j, :], in_=pb[:, 0:MEM]))
        else:
            for mt in range(2):
                pb = self.bank()
                def f(pb=pb, w=w, mt=mt):
                    for kc in range(8):
                        ins = nc.tensor.matmul(pb[:, :], memT[:, kc, mt * 128:(mt + 1) * 128], w[:, kc, :], start=(kc == 0), stop=(kc == 7))
                    return ins
                kb.op("pe", [pb], [w, memT], f, n=8)
                kb.op("act", [vm], [pb], lambda pb=pb, mt=mt, c0=(grp - 2) * 512: nc.scalar.copy(out=vm[:, mt, c0:c0 + 512], in_=pb[:, :]))
    xT_t = kb.sb("xa_xT", [128, 8, 512], BF16)
    qT = kb.sb("xa_qT", [128, 8, 512], BF16)
    oT = kb.sb("xa_oT", [128, 8, 512], BF16)
    pTm = [kb.sb(f"xa_pT{i}", [128, 512], BF16) for i in range(4)]
    rec = kb.sb("xa_rec", [128, 512], F32)
    xin = [kb.sb(f"xa_x{i}", [128, D], F32) for i in range(2)]
    hb = [kb.sb(f"xa_h{i}", [128, D], F32) for i in range(2)]
    ob = [kb.sb(f"xa_o{i}", [128, D], F32) for i in range(2)]
    cnt = [0]
    stg = [kb.sb(f"xa_stg{i}", [128, D], F32) for i in range(2)]
    for tg in range(8):
        self.build_xT(xsrc[tg * 512:(tg + 1) * 512, :], xT_t, ntiles=4, stg=stg)
        for j in range(8):
            pb = self.bank()
            def f(pb=pb, j=j):
                for kc in range(8):
                    ins = nc.tensor.matmul(pb[:, :], wq[:, kc, j * 128:(j + 1) * 128], xT_t[:, kc, :], start=(kc == 0), stop=(kc == 7))
                return ins
            kb.op("pe", [pb], [wq, xT_t], f, n=8)
            if j % 2 == 0:
                kb.op("act", [qT], [pb], lambda pb=pb, j=j: nc.scalar.activation(out=qT[:, j, :], in_=pb[:, :], func=AF.Copy, scale=1.0 / 16))
            else:
                kb.op("dve", [qT], [pb], lambda pb=pb, j=j: nc.vector.tensor_scalar(out=qT[:, j, :], in0=pb[:, :], scalar1=1.0 / 16, scalar2=None, op0=ALU.mult))
        for h in range(4):
            pts = []
            for mt in range(2):
                pb = self.bank()
                def f(pb=pb, h=h, mt=mt):
                    for c in range(2):
                        ins = nc.tensor.matmul(pb[:, :], kTm[:, 2 * h + c, mt * 128:(mt + 1) * 128], qT[:, 2 * h + c, :], start=(c == 0), stop=(c == 1))
                    return ins
                kb.op("pe", [pb], [kTm, qT], f, n=2)
                p_t = pTm[(h % 2) * 2 + mt]
                kb.op("act", [p_t], [pb], lambda pb=pb, p_t=p_t: nc.scalar.activation(out=p_t[:], in_=pb[:, :], func=AF.Exp))
                pts.append(p_t)
            pb = self.bank()
            def fs(pb=pb, pts=pts):
                for mt in range(2):
                    ins = nc.tensor.matmul(pb[:, :], self.onesb[:, :], pts[mt][:], start=(mt == 0), stop=(mt == 1))
                return ins
            kb.op("pe", [pb], [self.onesb] + pts, fs, n=2)
            kb.op("dve", [rec], [pb], lambda pb=pb: nc.vector.reciprocal(out=rec[:], in_=pb[:, :]))
            for c in range(2):
                pb = self.bank()
                def fo(pb=pb, pts=pts, h=h, c=c):
                    for mt in range(2):
                        ins = nc.tensor.matmul(pb[:, :], vm[:, mt, h * 256 + c * 128:h * 256 + (c + 1) * 128], pts[mt][:], start=(mt == 0), stop=(mt == 1))
                    return ins
                kb.op("pe", [pb], [vm] + pts, fo, n=2)
                kb.op("dve", [oT], [pb, rec], lambda pb=pb, h=h, c=c: nc.vector.tensor_tensor(out=oT[:, 2 * h + c, :], in0=pb[:, :], in1=rec[:], op=ALU.mult))
        self.proj_res_ln(L, oT, wo, xsrc, xdst, tg, xin, hb, ob, cnt)
    kb.pop()


Prog.p7_xattn = _xattn


def _moe(self, l, xsrc, xdst):
    kb, nc = self.kb, self.nc
    xs, ys = self.scr["moe_xs"], self.scr["moe_ys"]
    kb.push()
    didx = kb.sb("mo_didx", [128, NT, 4], I32)
    gts = kb.sb("mo_gts", [128, NT, 4], F32)
    kb.push()
    zt = kb.sb("mo_zero", [128, 8192], BF16)
    kb.op("pool", [zt], [], lambda: nc.gpsimd.memset(zt[:], 0.0))
    xsz = xs.rearrange("(a p r) d -> a p (r d)", p=128, r=8)
    for a in range(NE * CAP // 1024):
        kb.dma("sp" if a % 2 == 0 else "act", xsz[a], zt[:], [], [zt])
    kb.barrier()
    rw = kb.sb("mo_rw", [128, 8, NE], F32)
    kb.dma("sp", rw[:], self.din["router_w"][l].rearrange("(kc p) e -> p kc e", p=128), [rw], [])
    rb = kb.sb("mo_rb", [128, NE], F32)
    kb.dma("sp", rb[:], self.din["router_b"][l:l + 1, :].to_broadcast([128, NE]), [rb], [])
    eoff = kb.sb("mo_eoff", [128, NE], F32)
    for e in range(NE):
        kb.op("pool", [eoff], [], lambda e=e: nc.gpsimd.memset(eoff[:, e:e + 1], float(e * CAP)))
    lst = kb.sb("mo_lst", [128, 128], BF16)
    kb.op("pool", [lst], [], lambda: nc.gpsimd.memset(lst[:], 1.0))
    kb.op("pool", [lst], [lst], lambda: nc.gpsimd.affine_select(out=lst[:], in_=lst[:], pattern=[[1, 128]], compare_op=ALU.is_gt, fill=0.0, base=0, channel_multiplier=-1))
    off = kb.sb("mo_off", [128, NE], F32)
    kb.op("dve", [off], [], lambda: nc.vector.memset(off[:], 0.0))
    x_t = [kb.sb(f"mo_x{i}", [128, D], F32) for i in range(2)]
    xb = [kb.sb(f"mo_xb{i}", [128, D], BF16) for i in range(2)]
    xTf = kb.sb("mo_xTf", [128, 8, 128], F32)
    lg = kb.sb("mo_lg", [128, NE], F32)
    m8 = kb.sb("mo_m8", [128, 8], F32)
    msk = kb.sb("mo_msk", [128, NE], BF16)
    dest = kb.sb("mo_dest", [128, NE], F32)
    oh = kb.sb("mo_oh", [128, NE], F32)
    dk = kb.sb("mo_dk", [128, 4], F32)
    nm0 = kb.sb("mo_nm0", [128, 1], F32)
    e4 = kb.sb("mo_e4", [128, 4], F32)
    s4 = kb.sb("mo_s4", [128, 1], F32)
    for i in range(NT):
        xt, xbt = x_t[i % 2], xb[i % 2]
        kb.dma("sp", xt[:], xsrc[i * 128:(i + 1) * 128, :], [xt], [])
        kb.op("act", [xbt], [xt], lambda xt=xt, xbt=xbt: nc.scalar.copy(out=xbt[:], in_=xt[:]))
        for half in range(2):
            pb = self.bank()
            def f(pb=pb, xt=xt, half=half):
                for j in range(4):
                    kc = half * 4 + j
                    ins = nc.tensor.transpose(out=pb[:, j * 128:(j + 1) * 128], in_=xt[:, kc * 128:(kc + 1) * 128], identity=self.ident[:])
                return ins
            kb.op("pe", [pb], [xt, self.ident], f, n=4)
            kb.op("dve", [xTf], [pb], lambda pb=pb, half=half: nc.vector.tensor_copy(out=xTf[:, half * 4:half * 4 + 4, :], in_=pb[:].rearrange("p (j t) -> p j t", j=4)))
        pb = self.bank()
        def fr(pb=pb):
            for kc in range(8):
                ins = nc.tensor.matmul(pb[:, 0:NE], xTf[:, kc, :], rw[:, kc, :], start=(kc == 0), stop=(kc == 7))
            return ins
        kb.op("pe", [pb], [xTf, rw], fr, n=8)
        kb.op("dve", [lg], [pb, rb], lambda pb=pb: nc.vector.tensor_tensor(out=lg[:], in0=pb[:, 0:NE], in1=rb[:], op=ALU.add))
        kb.op("dve", [m8], [lg], lambda: nc.vector.max(out=m8[:], in_=lg[:]))
        kb.op("dve", [msk], [lg, m8], lambda: nc.vector.tensor_scalar(out=msk[:], in0=lg[:], scalar1=m8[:, 3:4], scalar2=None, op0=ALU.is_ge))
        pp = self.bank()
        kb.op("pe", [pp], [lst, msk], lambda pp=pp: nc.tensor.matmul(pp[:, 0:NE], lst[:, :], msk[:, :], start=True, stop=True))
        kb.op("dve", [dest], [pp, off], lambda pp=pp: nc.vector.tensor_tensor(out=dest[:], in0=pp[:, 0:NE], in1=off[:], op=ALU.add))
        kb.op("dve", [dest], [dest, eoff], lambda: nc.vector.scalar_tensor_tensor(out=dest[:], in0=dest[:], scalar=float(CAP - 1), in1=eoff[:], op0=ALU.min, op1=ALU.add))
        pc_ = self.bank()
        kb.op("pe", [pc_], [self.onesb, msk], lambda pc_=pc_: nc.tensor.matmul(pc_[:, 0:NE], self.onesb[:, :], msk[:, :], start=True, stop=True))
        kb.op("dve", [off], [off, pc_], lambda pc_=pc_: nc.vector.tensor_tensor(out=off[:], in0=off[:], in1=pc_[:, 0:NE], op=ALU.add))
        kb.op("dve", [nm0], [m8], lambda: nc.vector.tensor_scalar(out=nm0[:], in0=m8[:, 0:1], scalar1=-1.0, scalar2=None, op0=ALU.mult))
        kb.op("dve", [s4], [], lambda: nc.vector.memset(s4[:], 0.0))
        kb.op("act", [e4, s4], [m8, nm0], lambda: nc.scalar.activation(out=e4[:], in_=m8[:, 0:4], func=AF.Exp, bias=nm0[:, 0:1], scale=1.0, accum_out=s4[:, 0:1]))
        kb.op("dve", [s4], [s4], lambda: nc.vector.reciprocal(out=s4[:], in_=s4[:]))
        kb.op("dve", [gts], [e4, s4], lambda i=i: nc.vector.tensor_scalar(out=gts[:, i, :], in0=e4[:], scalar1=s4[:, 0:1], scalar2=None, op0=ALU.mult))
        for k in range(4):
            kb.op("dve", [oh], [lg, m8, dest], lambda k=k: nc.vector.scalar_tensor_tensor(out=oh[:], in0=lg[:], scalar=m8[:, k:k + 1], in1=dest[:], op0=ALU.is_equal, op1=ALU.mult))
            kb.op("dve", [dk], [oh], lambda k=k: nc.vector.tensor_reduce(out=dk[:, k:k + 1], in_=oh[:], axis=AX.X, op=ALU.add))
        kb.op("dve", [didx], [dk], lambda i=i: nc.vector.tensor_copy(out=didx[:, i, :], in_=dk[:]))
        for k in range(4):
            kb.idma(xs[:, :], bass.IndirectOffsetOnAxis(ap=didx[:, i, k:k + 1], axis=0), xbt[:, :], None, [], [xbt, didx])
    kb.pop()
    kb.push()
    bgr = kb.sb("mo_bgr", [NE, 2 * D], F32)
    kb.dma("sp", bgr[:], self.din["expert_b_gu"][l], [bgr], [])
    bgu = kb.sb("mo_bgu", [128, 16, NE], F32)
    for half in range(2):
        pb = self.bank()
        def fb(pb=pb, half=half):
            for j in range(8):
                c = half * 8 + j
                ins = nc.tensor.transpose(out=pb[:, j * NE:(j + 1) * NE], in_=bgr[:, c * 128:(c + 1) * 128], identity=self.ident[0:NE, 0:NE])
            return ins
        kb.op("pe", [pb], [bgr, self.ident], fb, n=8)
        kb.op("dve", [bgu], [pb], lambda pb=pb, half=half: nc.vector.tensor_copy(out=bgu[:, half * 8:half * 8 + 8, :], in_=pb[:, 0:8 * NE].rearrange("p (c e) -> p c e", c=8)))
    wgu = [kb.sb(f"mo_wgu{i}", [128, 8, 2 * D], BF16) for i in range(2)]
    wdn = [kb.sb(f"mo_wdn{i}", [128, 8, D], BF16) for i in range(2)]
    bdn = [kb.sb(f"mo_bdn{i}", [128, D], F32) for i in range(2)]
    xse = [kb.sb(f"mo_xse{i}", [128, D], BF16) for i in range(2)]
    xsT = kb.sb("mo_xsT", [128, 8, CAP], BF16)
    hT = kb.sb("mo_hT", [128, 8, CAP], BF16)
    gp = kb.sb("mo_gp", [128, 512], F32)
    sgm = kb.sb("mo_sgm", [128, 512], F32)
    up = kb.sb("mo_up", [128, 512], F32)
    yst = [kb.sb(f"mo_ys{i}", [128, D], F32) for i in range(2)]
    yc = 0
    for e in range(NE):
        wg, wd, bd = wgu[e % 2], wdn[e % 2], bdn[e % 2]
        wgv = self.din["expert_w_gu"][l, e].rearrange("(kc p) n -> p kc n", p=128)
        kb.dma("pool", wg[:, 0:4, :], wgv[:, 0:4, :], [wg], [])
        kb.dma("pool", wg[:, 4:8, :], wgv[:, 4:8, :], [wg], [])
        kb.dma("pool", wd[:], self.din["expert_w_down"][l, e].rearrange("(kc p) n -> p kc n", p=128), [wd], [])
        kb.dma("sp", bd[:], self.din["expert_b_down"][l, e:e + 1, :].to_broadcast([128, D]), [bd], [])
        for st in range(CAP // 128):
            xt = xse[st % 2]
            kb.dma("sp", xt[:], xs[e * CAP + st * 128:e * CAP + (st + 1) * 128, :], [xt], [])
            pt = self.ps_bf
            def ft(pt=pt, xt=xt):
                for kc in range(8):
                    ins = nc.tensor.transpose(out=pt[:, kc * 128:(kc + 1) * 128], in_=xt[:, kc * 128:(kc + 1) * 128], identity=self.identb[:, :])
                return ins
            kb.op("pe", [pt], [xt, self.identb], ft, n=8)
            if st % 2 == 0:
                kb.op("act", [xsT], [pt], lambda pt=pt, st=st: nc.scalar.copy(out=xsT[:, :, st * 128:(st + 1) * 128], in_=pt[:, :].rearrange("p (k t) -> p k t", k=8)))
            else:
                kb.op("dve", [xsT], [pt], lambda pt=pt, st=st: nc.vector.tensor_copy(out=xsT[:, :, st * 128:(st + 1) * 128], in_=pt[:, :].rearrange("p (k t) -> p k t", k=8)))
        for c in range(8):
            for (s0, sn) in ((0, 512), (512, CAP - 512)):
                pg, pu = self.bank(), self.bank()
                def fg(pg=pg, wg=wg, c=c, s0=s0, sn=sn):
                    for kc in range(8):
                        ins = nc.tensor.matmul(pg[:, 0:sn], wg[:, kc, c * 128:(c + 1) * 128], xsT[:, kc, s0:s0 + sn], start=(kc == 0), stop=(kc == 7))
                    return ins
                kb.op("pe", [pg], [wg, xsT], fg, n=8)
                def fu(pu=pu, wg=wg, c=c, s0=s0, sn=sn):
                    for kc in range(8):
                        ins = nc.tensor.matmul(pu[:, 0:sn], wg[:, kc, D + c * 128:D + (c + 1) * 128], xsT[:, kc, s0:s0 + sn], start=(kc == 0), stop=(kc == 7))
                    return ins
                kb.op("pe", [pu], [wg, xsT], fu, n=8)
                kb.op("dve", [gp], [pg, bgu], lambda pg=pg, c=c, sn=sn, e=e: nc.vector.tensor_scalar(out=gp[:, 0:sn], in0=pg[:, 0:sn], scalar1=bgu[:, c, e:e + 1], scalar2=7.0, op0=ALU.add, op1=ALU.min))
                kb.op("act", [sgm], [gp], lambda sn=sn: nc.scalar.activation(out=sgm[:, 0:sn], in_=gp[:, 0:sn], func=AF.Sigmoid, scale=1.702))
                kb.op("dve", [up], [pu, bgu], lambda pu=pu, c=c, sn=sn, e=e: nc.vector.tensor_scalar(out=up[:, 0:sn], in0=pu[:, 0:sn], scalar1=bgu[:, 8 + c, e:e + 1], scalar2=-7.0, op0=ALU.add, op1=ALU.max))
                kb.op("pool", [up], [up], lambda sn=sn: nc.gpsimd.tensor_scalar(out=up[:, 0:sn], in0=up[:, 0:sn], scalar1=7.0, scalar2=1.0, op0=ALU.min, op1=ALU.add))
                kb.op("pool", [gp], [gp, sgm], lambda sn=sn: nc.gpsimd.tensor_tensor(out=gp[:, 0:sn], in0=gp[:, 0:sn], in1=sgm[:, 0:sn], op=ALU.mult))
                kb.op("dve", [hT], [gp, up], lambda c=c, s0=s0, sn=sn: nc.vector.tensor_tensor(out=hT[:, c, s0:s0 + sn], in0=gp[:, 0:sn], in1=up[:, 0:sn], op=ALU.mult))
        for st in range(CAP // 128):
            y_t = yst[yc % 2]
            yc += 1
            for half in range(2):
                pd = self.bank()
                def fd(pd=pd, wd=wd, st=st, half=half):
                    for c in range(8):
                        ins = nc.tensor.matmul(pd[:, :], hT[:, c, st * 128:(st + 1) * 128], wd[:, c, half * 512:(half + 1) * 512], start=(c == 0), stop=(c == 7))
                    return ins
                kb.op("pe", [pd], [hT, wd], fd, n=8)
                kb.op("dve", [y_t], [pd, bd], lambda pd=pd, half=half, y_t=y_t, bd=bd: nc.vector.tensor_tensor(out=y_t[:, half * 512:(half + 1) * 512], in0=pd[:, :], in1=bd[:, half * 512:(half + 1) * 512], op=ALU.add))
            kb.dma("sp", ys[e * CAP + st * 128:e * CAP + (st + 1) * 128, :], y_t[:], [], [y_t])
    kb.pop()
    kb.push()
    L = self.ln_setup(l, 2)
    gk = [kb.sb(f"mo_gk{i}", [128, D], F32) for i in range(4)]
    xin = [kb.sb(f"mo_cx{i}", [128, D], F32) for i in range(2)]
    acc = [kb.sb(f"mo_acc{i}", [128, D], F32) for i in range(2)]
    ob = [kb.sb(f"mo_co{i}", [128, D], F32) for i in range(2)]
    for i in range(NT):
        a_t, x_in, o_t = acc[i % 2], xin[i % 2], ob[i % 2]
        kb.dma("sp", x_in[:], xsrc[i * 128:(i + 1) * 128, :], [x_in], [])
        for k in range(4):
            kb.idma(gk[k][:, :], None, ys[:, :], bass.IndirectOffsetOnAxis(ap=didx[:, i, k:k + 1], axis=0), [gk[k]], [didx])
        kb.op("dve", [a_t], [x_in, gk[0], gts], lambda a_t=a_t, x_in=x_in, i=i: nc.vector.tensor_scalar(out=a_t[:], in0=gk[0][:], scalar1=gts[:, i, 0:1], scalar2=None, op0=ALU.mult))
        for k in range(1, 4):
            kb.op("dve", [a_t], [a_t, gk[k], gts], lambda a_t=a_t, k=k, i=i: nc.vector.scalar_tensor_tensor(out=a_t[:], in0=gk[k][:], scalar=gts[:, i, k:k + 1], in1=a_t[:], op0=ALU.mult, op1=ALU.add))
        kb.op("dve", [a_t], [a_t, x_in], lambda a_t=a_t, x_in=x_in: nc.vector.scalar_tensor_tensor(out=a_t[:], in0=x_in[:], scalar=ALPHA, in1=a_t[:], op0=ALU.mult, op1=ALU.add))
        self.ln_tile(L, a_t, o_t)
        kb.dma("sp", xdst[i * 128:(i + 1) * 128, :], o_t[:], [], [o_t])
    kb.pop()
    kb.pop()


Prog.p8_moe = _moe


def build_program(dbg=()):
    P = Prog(dbg=dbg)
    P.alloc_scratch()
    P.consts()
    P.bias_setup()
    xcur = P.din["x"]
    for l in range(DEPTH):
        P.p1_inproj(l, xcur)
        P.p23_nsa(l)
        P.p4_conv(l)
        P.p5_gla(l)
        P.p6_merge(l, xcur, P.scr["x1"])
        P.p7_xattn(l, P.scr["x1"], P.scr["x2"])
        last = (l == DEPTH - 1)
        P.p8_moe(l, P.scr["x2"], P.y if last else P.scr["x3"])
        xcur = P.scr["x3"]
    P.finish()
    return P


def make_in_maps(inputs, n=8):
    consts = {"c_ident": np.eye(128, dtype=np.float32), "c_tab": _const_tab(), "c_esel": _const_esel()}
    x = np.asarray(inputs["x"], np.float32)
    mem = np.asarray(inputs["mem"], np.float32)
    shared = {k: np.ascontiguousarray(np.asarray(inputs[k], np.float32)) for k in WNAMES}
    maps = []
    for b in range(n):
        m = {"x": np.ascontiguousarray(x[b]), "mem": np.ascontiguousarray(mem[b])}
        m.update(shared)
        m.update(consts)
        maps.append(m)
    return maps


def kernel(**inputs):
    P = build_program()
    maps = make_in_maps(inputs, 8)
    res = run_bass_kernel_spmd(P.nc, maps, core_ids=list(range(8)))
    out = np.stack([np.asarray(res.results[b]["y"], np.float32) for b in range(8)], axis=0)
    return out
```

```python
import math
from contextlib import ExitStack

import numpy as np
import concourse.bass as bass
import concourse.mybir as mybir
from concourse.bass_utils import run_bass_kernel_spmd

F32 = mybir.dt.float32
BF16 = mybir.dt.bfloat16
I32 = mybir.dt.int32
AF = mybir.ActivationFunctionType
ALU = mybir.AluOpType
AX = mybir.AxisListType

D = 1024
S = 4096
NT = S // 128
DEPTH = 2
MEM = 256
D_IN = 6952
O_Q, O_KV, O_NG, O_CONV, O_GQ, O_GK, O_GV, O_GA, O_GR, O_MG = 0, 512, 1280, 1304, 2328, 2584, 2840, 3352, 3368, 3880
ALPHA = (2 * DEPTH) ** 0.25
NE = 32
CAP = 768
NEG = -30000.0


class Ev:
    __slots__ = ("key", "val", "snap")

    def __init__(self, key, val, snap):
        self.key, self.val, self.snap = key, val, snap


class Tk:
    __slots__ = ("w", "r", "name")

    def __init__(self, name=""):
        self.w = None
        self.r = {}
        self.name = name


class T:
    def __init__(self, h, name):
        self.h = h
        self.tk = Tk(name)

    def __getitem__(self, idx):
        return self.h[idx]


class KB:
    NDS = 8

    def __init__(self, nc):
        self.nc = nc
        self.es = ExitStack()
        self.E = {"pe": nc.tensor, "act": nc.scalar, "dve": nc.vector, "pool": nc.gpsimd, "sp": nc.sync}
        self.sem = {}
        self.cnt = {}
        self.known = {e: {} for e in self.E}
        for e in self.E:
            self.sem["c_" + e] = self.es.enter_context(nc.semaphore("c_" + e))
            self.cnt["c_" + e] = 0
        self.dring = {}
        for q in ("sp", "pool", "act"):
            ring = []
            for i in range(self.NDS):
                k = f"d_{q}{i}"
                self.sem[k] = self.es.enter_context(nc.semaphore(k))
                self.cnt[k] = 0
                ring.append(k)
            self.dring[q] = [ring, 0, {}]
        self.ninst = 0
        self.tiles = []

    def sb(self, name, shape, dt):
        self.uid = getattr(self, "uid", 0) + 1
        name = f"{name}_{self.uid}"
        t = T(self.es.enter_context(self.nc.sbuf_tensor(name, list(shape), dt)), name)
        self.tiles.append(t)
        return t

    def ps(self, name, shape, dt=F32):
        t = T(self.es.enter_context(self.nc.psum_tensor(name, list(shape), dt)), name)
        t.is_psum = True
        self.tiles.append(t)
        return t

    def _wait(self, e, ev):
        if ev is None:
            return
        kn = self.known[e]
        if kn.get(ev.key, 0) >= ev.val:
            return
        self.E[e].wait_ge(self.sem[ev.key], ev.val)
        self.ninst += 1
        for k, v in ev.snap.items():
            if kn.get(k, 0) < v:
                kn[k] = v
        kn[ev.key] = ev.val

    def _deps(self, e, outs, ins):
        for t in ins:
            tk = t.tk if isinstance(t, T) else t
            self._wait(e, tk.w)
        for t in outs:
            tk = t.tk if isinstance(t, T) else t
            self._wait(e, tk.w)
            for r in list(tk.r.values()):
                self._wait(e, r)

    def _record(self, ev, outs, ins):
        for t in ins:
            tk = t.tk if isinstance(t, T) else t
            tk.r[ev.key] = ev
        for t in outs:
            tk = t.tk if isinstance(t, T) else t
            tk.w = ev
            tk.r = {}

    def op(self, e, outs, ins, fn, n=1):
        outs = list(outs) + [t for t in ins if getattr(t, "is_psum", False) and t not in outs]
        self._deps(e, outs, ins)
        inst = fn()
        key = "c_" + e
        self.cnt[key] += 1
        inst.then_inc(self.sem[key], 1)
        self.ninst += n
        ev = Ev(key, self.cnt[key], dict(self.known[e]))
        self._record(ev, outs, ins)
        return ev

    def dma(self, q, out, in_, outs, ins, **kw):
        e = q
        ring, idx, last = self.dring[q]
        key = ring[idx % self.NDS]
        self.dring[q][1] = idx + 1
        self._deps(e, outs, ins)
        self._wait(e, last.get(key))
        inst = self.E[e].dma_start(out=out, in_=in_, **kw)
        self.cnt[key] += 16
        inst.then_inc(self.sem[key], 16)
        self.ninst += 1
        ev = Ev(key, self.cnt[key], dict(self.known[e]))
        last[key] = ev
        self._record(ev, outs, ins)
        return ev

    def idma(self, out, out_off, in_, in_off, outs, ins, **kw):
        e = "pool"
        ring, idx, last = self.dring[e]
        key = ring[idx % self.NDS]
        self.dring[e][1] = idx + 1
        self._deps(e, outs, ins)
        self._wait(e, last.get(key))
        inst = self.nc.gpsimd.indirect_dma_start(out=out, out_offset=out_off, in_=in_, in_offset=in_off, **kw)
        self.cnt[key] += 16
        inst.then_inc(self.sem[key], 16)
        self.ninst += 1
        ev = Ev(key, self.cnt[key], dict(self.known[e]))
        last[key] = ev
        self._record(ev, outs, ins)
        return ev

    def barrier(self, extra=()):
        evs = []
        for e in self.E:
            key = "c_" + e
            if self.cnt[key]:
                evs.append(Ev(key, self.cnt[key], {}))
        for q in self.dring:
            for key, ev in self.dring[q][2].items():
                evs.append(Ev(key, self.cnt[key], {}))
        for e in self.E:
            for ev in evs:
                self._wait(e, ev)
        for t in self.tiles:
            t.tk.w = None
            t.tk.r = {}
        for tk in extra:
            tk.w = None
            tk.r = {}

    def push(self):
        self._saved = getattr(self, "_saved", [])
        self._saved.append((self.es, self.tiles))
        self.es = ExitStack()
        self.tiles = list(self.tiles)

    def pop(self):
        self.barrier()
        self.es.close()
        self.es, self.tiles = self._saved.pop()


WNAMES = ["rel_bias", "w_in", "cmp_pe", "cmp_w1", "cmp_b1", "cmp_w2", "conv_w", "conv_b", "conv_norm_g",
          "conv_norm_b", "gla_gate_w", "gla_gate_b", "gla_norm_g", "w_branch", "w_out", "xa_wq", "xa_wkv", "xa_wo",
          "router_w", "router_b", "expert_w_gu", "expert_b_gu", "expert_w_down", "expert_b_down", "norm_g", "norm_b"]
WSHAPES = {
    "rel_bias": (32, 8), "w_in": (2, 1024, 6952), "cmp_pe": (2, 2, 32, 64), "cmp_w1": (2, 2, 32, 64, 64),
    "cmp_b1": (2, 2, 64), "cmp_w2": (2, 2, 64, 64), "conv_w": (2, 31, 512), "conv_b": (2, 512),
    "conv_norm_g": (2, 512), "conv_norm_b": (2, 512), "gla_gate_w": (2, 16, 256), "gla_gate_b": (2, 256),
    "gla_norm_g": (2, 128), "w_branch": (2, 3, 512, 1024), "w_out": (2, 1024, 1024), "xa_wq": (2, 1024, 1024),
    "xa_wkv": (2, 1024, 2048), "xa_wo": (2, 1024, 1024), "router_w": (2, 1024, 32), "router_b": (2, 32),
    "expert_w_gu": (2, 32, 1024, 2048), "expert_b_gu": (2, 32, 2048), "expert_w_down": (2, 32, 1024, 1024),
    "expert_b_down": (2, 32, 1024), "norm_g": (2, 3, 1024), "norm_b": (2, 3, 1024),
}


class Prog:
    def __init__(self, dbg=(), use=None):
        self.dbg = set(dbg)
        nc = self.nc = bass.Bass("TRN2", target_bir_lowering=False)
        self.kb = KB(nc)
        self.din = {}
        self.din["x"] = nc.dram_tensor("x", [S, D], F32, kind="ExternalInput").ap()
        self.din["mem"] = nc.dram_tensor("mem", [MEM, D], F32, kind="ExternalInput").ap()
        for n in WNAMES:
            if use is None or n in use:
                self.din[n] = nc.dram_tensor(n, list(WSHAPES[n]), F32, kind="ExternalInput").ap()
        self.din["c_ident"] = nc.dram_tensor("c_ident", [128, 128], F32, kind="ExternalInput").ap()
        self.din["c_tab"] = nc.dram_tensor("c_tab", [128, C_TAB_W], F32, kind="ExternalInput").ap()
        self.din["c_esel"] = nc.dram_tensor("c_esel", [64, 32 * 128], F32, kind="ExternalInput").ap()
        self.y = nc.dram_tensor("y", [S, D], F32, kind="ExternalOutput").ap()
        self.scr = {}

    def dram(self, name, shape, dt):
        kind = "ExternalOutput" if name in self.dbg else "Internal"
        ap = self.nc.dram_tensor(name, list(shape), dt, kind=kind).ap()
        self.scr[name] = ap
        return ap

    def consts(self):
        kb, nc = self.kb, self.nc
        self.ident = kb.sb("ident", [128, 128], F32)
        kb.dma("sp", self.ident[:], self.din["c_ident"][:, :], [self.ident], [])
        self.identb = kb.sb("identb", [128, 128], BF16)
        kb.op("dve", [self.identb], [self.ident], lambda: nc.vector.tensor_copy(out=self.identb[:], in_=self.ident[:]))
        self.onesb = kb.sb("onesb", [128, 128], BF16)
        kb.op("dve", [self.onesb], [], lambda: nc.vector.memset(self.onesb[:], 1.0))
        self.onesf = kb.sb("onesf", [128, 128], F32)
        kb.op("dve", [self.onesf], [], lambda: nc.vector.memset(self.onesf[:], 1.0))
        self.eps5 = kb.sb("eps5", [128, 1], F32)
        kb.op("dve", [self.eps5], [], lambda: nc.vector.memset(self.eps5[:], 1e-5))
        self.eps6 = kb.sb("eps6", [128, 1], F32)
        kb.op("dve", [self.eps6], [], lambda: nc.vector.memset(self.eps6[:], 1e-6))
        self.psb = [kb.ps(f"psb{i}", [128, 512], F32) for i in range(7)]
        self.ps_bf = kb.ps("ps_bf", [128, 1024], BF16)
        self.psi = 0
        self.nrot = 7

    def bank(self):
        b = self.psb[self.psi % self.nrot]
        self.psi += 1
        return b

    def build_xT(self, src, xT, ntiles=NT, dt_out=BF16, stg=None):
        kb, nc = self.kb, self.nc
        if stg is None:
            stg = [kb.sb(f"xstg{i}", [128, D], F32) for i in range(2)]
        for i in range(ntiles):
            st = stg[i % 2]
            kb.dma("sp", st[:], src[i * 128:(i + 1) * 128, :], [st], [])
            for half in range(2):
                pb = self.bank()
                def f(pb=pb, st=st, half=half):
                    for j in range(4):
                        kc = half * 4 + j
                        ins = nc.tensor.transpose(out=pb[:, j * 128:(j + 1) * 128], in_=st[:, kc * 128:(kc + 1) * 128], identity=self.ident[:])
                    return ins
                kb.op("pe", [pb], [st, self.ident], f, n=4)
                eng = "act" if half == 0 else "dve"
                dst = xT[:, half * 4:half * 4 + 4, i * 128:(i + 1) * 128]
                srcp = pb[:].rearrange("p (j t) -> p j t", j=4)
                if eng == "act":
                    kb.op("act", [xT], [pb], lambda dst=dst, srcp=srcp: nc.scalar.copy(out=dst, in_=srcp))
                else:
                    kb.op("dve", [xT], [pb], lambda dst=dst, srcp=srcp: nc.vector.tensor_copy(out=dst, in_=srcp))

    def p1_inproj(self, l, xsrc, parts=("fm", "tm"), ngrp_lim=None):
        kb, nc = self.kb, self.nc
        zT = self.scr["zT"]
        w_in = self.din["w_in"]
        kb.push()
        xT = kb.sb("xT", [128, 8, S], BF16)
        self.build_xT(xsrc, xT)
        wv = w_in[l].rearrange("(kc p) n -> p kc n", p=128)
        wb = [kb.sb(f"p1w{i}", [128, 8, 512], BF16) for i in range(2)]
        zst = [kb.sb(f"p1z{i}", [128, S], F32) for i in range(2)]
        ngrp = (D_IN + 511) // 512
        if ngrp_lim:
            ngrp = ngrp_lim
        ci = 0
        for g in range(ngrp if "fm" in parts else 0):
            c0 = g * 512
            ncol = min(512, D_IN - c0)
            w = wb[g % 2]
            kb.dma("pool", w[:, :, 0:ncol], wv[:, :, c0:c0 + ncol], [w], [])
            for cc in range(0, ncol, 128):
                m = min(128, ncol - cc)
                z = zst[ci % 2]
                ci += 1
                for tg in range(8):
                    pb = self.bank()
                    def f(pb=pb, w=w, cc=cc, m=m, tg=tg):
                        for kc in range(8):
                            ins = nc.tensor.matmul(pb[0:m, :], w[:, kc, cc:cc + m], xT[:, kc, tg * 512:(tg + 1) * 512],
                                                   start=(kc == 0), stop=(kc == 7))
                        return ins
                    kb.op("pe", [pb], [w, xT], f, n=8)
                    if tg % 2 == 0:
                        kb.op("act", [z], [pb], lambda pb=pb, z=z, m=m, tg=tg: nc.scalar.copy(out=z[0:m, tg * 512:(tg + 1) * 512], in_=pb[0:m, :]))
                    else:
                        kb.op("dve", [z], [pb], lambda pb=pb, z=z, m=m, tg=tg: nc.vector.tensor_copy(out=z[0:m, tg * 512:(tg + 1) * 512], in_=pb[0:m, :]))
                kb.dma("sp", zT[c0 + cc:c0 + cc + m, :], z[0:m, :], [], [z])
        wt = kb.sb("p1wt", [128, 8, 792], BF16)
        for (o, n, d0) in ((896, 128, 0), (1152, 152, 128), (O_GV, 512, 280)):
            kb.dma("pool", wt[:, :, d0:d0 + n], wv[:, :, o:o + n], [wt], [])
        vst = kb.sb("p1v", [128, NT, 256], BF16)
        gst = kb.sb("p1g", [128, NT, 24], F32)
        gvst = kb.sb("p1gv", [128, NT, 512], BF16)
        for i in range(NT if "tm" in parts else 0):
            pa = self.bank()
            pg = self.bank()
            def fa(pa=pa, i=i):
                for kc in range(8):
                    ins = nc.tensor.matmul(pa[:, 0:280], xT[:, kc, i * 128:(i + 1) * 128], wt[:, kc, 0:280], start=(kc == 0), stop=(kc == 7))
                return ins
            kb.op("pe", [pa], [wt, xT], fa, n=8)
            def fg(pg=pg, i=i):
                for kc in range(8):
                    ins = nc.tensor.matmul(pg[:, :], xT[:, kc, i * 128:(i + 1) * 128], wt[:, kc, 280:792], start=(kc == 0), stop=(kc == 7))
                return ins
            kb.op("pe", [pg], [wt, xT], fg, n=8)
            kb.op("dve", [vst], [pa], lambda pa=pa, i=i: nc.vector.tensor_copy(out=vst[:, i, :], in_=pa[:, 0:256]))
            kb.op("act", [gst], [pa], lambda pa=pa, i=i: nc.scalar.activation(out=gst[:, i, :], in_=pa[:, 256:280], func=AF.Sigmoid))
            kb.op("act", [gvst], [pg], lambda pg=pg, i=i: nc.scalar.copy(out=gvst[:, i, :], in_=pg[:, :]))
        kb.dma("sp", self.scr["vtm"][:, :], vst[:].rearrange("p i c -> p (i c)"), [], [vst])
        kb.dma("sp", self.scr["ngate"][:, :], gst[:].rearrange("p i c -> p (i c)"), [], [gst])
        kb.dma("sp", self.scr["gvtm"][:, :], gvst[:].rearrange("p i c -> p (i c)"), [], [gvst])
        kb.pop()

    def alloc_scratch(self):
        self.dram("zT", [D_IN, S], F32)
        self.dram("vtm", [128, NT * 256], BF16)
        self.dram("ngate", [128, NT * 24], F32)
        self.dram("gvtm", [128, NT * 512], BF16)
        for i in range(3):
            self.dram(f"brT{i}", [512, S], BF16)
        for nm in ("x1", "x2", "x3"):
            self.dram(nm, [S, D], F32)
        self.dram("moe_xs", [NE * CAP, D], BF16)
        self.dram("moe_ys", [NE * CAP, D], F32)
        if "kcT" in self.dbg:
            self.dram("kcT", [64, 512], BF16)
            self.dram("vc", [128, 260], BF16)

    def finish(self):
        self.kb.barrier()

    def bias_setup(self):
        kb, nc = self.kb, self.nc
        rb = kb.sb("rb_rep", [128, 32, 8], F32)
        src = self.din["rel_bias"].rearrange("(o b) h -> o (b h)", o=1).to_broadcast([128, 256])
        kb.dma("sp", rb[:].rearrange("p b h -> p (b h)"), src, [rb], [])
        self.ndelta = kb.sb("ndelta", [128, 32, 8], F32)
        kb.op("dve", [self.ndelta], [rb], lambda: nc.vector.tensor_tensor(out=self.ndelta[:, 1:32, :], in0=rb[:, 0:31, :], in1=rb[:, 1:32, :], op=ALU.subtract))

    def make_bias_table(self, dist_t, dist, n, outs, tmp):
        kb, nc = self.kb, self.nc
        dt_, m0, mk = tmp
        kb.op("dve", [m0], [dist_t], lambda: nc.vector.tensor_scalar(out=m0[:, 0:n], in0=dist[:, 0:n], scalar1=0.0, scalar2=NEG, op0=ALU.is_lt, op1=ALU.mult))
        for h in range(8):
            o_t, o_ap = outs[h]
            kb.op("pool", [o_t], [m0], lambda o_ap=o_ap: nc.gpsimd.tensor_copy(out=o_ap, in_=m0[:, 0:n]))
        for b in range(1, 32):
            thr = float(T5_THR[b])
            kb.op("dve", [mk], [dist_t], lambda thr=thr: nc.vector.tensor_scalar(out=mk[:, 0:n], in0=dist[:, 0:n], scalar1=thr, scalar2=None, op0=ALU.is_lt))
            for h in range(8):
                o_t, o_ap = outs[h]
                eng = "dve" if h % 2 == 0 else "dve"
                kb.op(eng, [o_t], [mk, self.ndelta], lambda o_ap=o_ap, b=b, h=h: nc.vector.scalar_tensor_tensor(
                    out=o_ap, in0=mk[:, 0:n], scalar=self.ndelta[:, b, h:h + 1], in1=o_ap, op0=ALU.mult, op1=ALU.add))


def _t5_thresholds():
    n = np.arange(0, 4096, dtype=np.int32)
    exact = 16
    lr = np.log(np.maximum(n, 1).astype(np.float32) / np.float32(exact)) / np.float32(math.log(128 / exact))
    large = exact + (lr * np.float32(32 - exact)).astype(np.int32)
    bucket = np.where(n < exact, n, np.minimum(large, 31))
    thr = np.zeros(32, np.int64)
    for b in range(1, 32):
        thr[b] = int(np.min(n[bucket >= b]))
    return thr, bucket


T5_THR, T5_BUCKET = _t5_thresholds()


def _const_tab():
    q = np.arange(128, dtype=np.float32)[:, None]
    u = np.arange(504, dtype=np.float32)[None, :]
    dist_c = q - 16.0 * (u - 248.0) - 31.0
    k = np.arange(128, dtype=np.float32)[:, None]
    qq = np.arange(128, dtype=np.float32)[None, :]
    dist0 = qq - k
    dist1 = 128.0 + qq - k
    m4 = np.where(qq < k, 0.0, NEG).astype(np.float32)
    rel = np.arange(-62, 64)[None, :]
    ql = np.arange(128)[:, None]
    cur_off = (ql >= 64).astype(np.int64)
    d = rel - cur_off
    ftab = np.where(d > 0, -1.0e4, np.where(d == 0, 2.0e4, np.where(d == -1, 1.0e4, 0.0))).astype(np.float32)
    tab = np.concatenate([dist_c, dist0, dist1, m4, ftab], axis=1).astype(np.float32)
    return np.ascontiguousarray(tab)


def _const_esel():
    e = np.zeros((64, 32, 128), np.float32)
    for kt in range(32):
        e[2 * kt, kt, 0:64] = 1.0
        e[2 * kt + 1, kt, 64:128] = 1.0
    return e.reshape(64, 32 * 128)


C_TAB_W = 504 + 128 * 3 + 126


def _nsa(self, l):
    kb, nc = self.kb, self.nc
    zT = self.scr["zT"]
    kb.push()
    tab = kb.sb("ctab", [128, C_TAB_W], F32)
    kb.dma("sp", tab[:], self.din["c_tab"][:, :], [tab], [])
    esel = kb.sb("esel", [64, 32, 128], BF16)
    kb.dma("pool", esel[:].rearrange("p a b -> p (a b)"), self.din["c_esel"][:, :], [esel], [])
    Wc = [kb.sb(f"Wc{g}", [128, 4, 504], F32) for g in range(2)]
    BT0 = [kb.sb(f"BT0{g}", [128, 4, 128], F32) for g in range(2)]
    BT1 = [kb.sb(f"BT1{g}", [128, 4, 128], F32) for g in range(2)]
    tmp = (None, kb.sb("btm0", [128, 504], F32), kb.sb("btmk", [128, 504], F32))
    self.make_bias_table(tab, tab[:, 0:504], 504, [(Wc[h // 4], Wc[h // 4][:, h % 4, :]) for h in range(8)], tmp)
    self.make_bias_table(tab, tab[:, 504:632], 128, [(BT0[h // 4], BT0[h // 4][:, h % 4, :]) for h in range(8)], tmp)
    self.make_bias_table(tab, tab[:, 632:760], 128, [(BT1[h // 4], BT1[h // 4][:, h % 4, :]) for h in range(8)], tmp)
    BT4 = kb.sb("BT4", [128, 4, 128], F32)
    for hg in range(4):
        kb.op("pool", [BT4], [tab], lambda hg=hg: nc.gpsimd.tensor_copy(out=BT4[:, hg, :], in_=tab[:, 760:888]))
    kcT = kb.sb("kcT_sb", [64, 2, 256], BF16)
    vc = kb.sb("vc_sb", [128, 2, 2, 65], BF16)
    kb.op("dve", [kcT], [], lambda: nc.vector.memset(kcT[:], 0.0))
    kb.op("dve", [vc], [], lambda: nc.vector.memset(vc[:], 0.0))
    kb.op("dve", [vc], [], lambda: nc.vector.memset(vc[:, :, :, 64:65], 1.0))
    kv = [kb.sb(f"cmpkv{i}", [64, S], BF16) for i in range(2)]
    w1a = kb.sb("cmpw1", [64, 32, 64], BF16)
    pe_sb = kb.sb("cmppe", [32, 64], F32)
    peT = kb.sb("cmppeT", [64, 32], BF16)
    b1 = kb.sb("cmpb1", [64, 1], F32)
    cb = kb.sb("cmpcb", [64, 1], F32)
    w2 = kb.sb("cmpw2", [64, 64], BF16)
    xs = kb.sb("cmpxs", [64, 256], F32)
    x2 = kb.sb("cmpx2", [64, 256], F32)
    sg = kb.sb("cmpsg", [64, 256], F32)
    hT = kb.sb("cmphT", [64, 256], BF16)
    kb.op("dve", [hT], [], lambda: nc.vector.memset(hT[:], 0.0))
    for i in range(2):
        kb.dma("pool", w1a[:], self.din["cmp_w1"][l, i].rearrange("l d e -> d l e"), [w1a], [])
        kb.dma("sp", pe_sb[:], self.din["cmp_pe"][l, i], [pe_sb], [])
        kb.dma("sp", b1[:], self.din["cmp_b1"][l, i].rearrange("(e o) -> e o", o=1), [b1], [])
        kb.dma("pool", w2[:], self.din["cmp_w2"][l, i], [w2], [])
        pb = self.bank()
        kb.op("pe", [pb], [pe_sb, self.ident], lambda pb=pb: nc.tensor.transpose(out=pb[0:64, 0:32], in_=pe_sb[:, :], identity=self.ident[0:32, 0:32]))
        kb.op("dve", [peT], [pb], lambda pb=pb: nc.vector.tensor_copy(out=peT[:], in_=pb[0:64, 0:32]))
        pb = self.bank()
        def fc0(pb=pb):
            for ll in range(32):
                ins = nc.tensor.matmul(pb[0:64, 0:1], w1a[:, ll, :], peT[:, ll:ll + 1], start=(ll == 0), stop=(ll == 31))
            return ins
        kb.op("pe", [pb], [w1a, peT], fc0, n=32)
        kb.op("dve", [cb], [pb, b1], lambda pb=pb: nc.vector.tensor_tensor(out=cb[:], in0=pb[0:64, 0:1], in1=b1[:], op=ALU.add))
        for g in range(2):
            kvt = kv[g]
            r0 = O_KV + i * 128 + g * 64
            kb.dma("pool", kvt[:], zT[r0:r0 + 64, :], [kvt], [])
            kvr = kvt[:].rearrange("p (c s) -> p c s", s=16)
            pb = self.bank()
            def facc(pb=pb, kvr=kvr):
                for ll in range(32):
                    c0, s_ = (0, ll) if ll < 16 else (1, ll - 16)
                    ins = nc.tensor.matmul(pb[0:64, 0:255], w1a[:, ll, :], kvr[:, c0:c0 + 255, s_], start=(ll == 0), stop=(ll == 31))
                return ins
            kb.op("pe", [pb], [w1a, kvt], facc, n=32)
            kb.op("act", [xs], [pb, cb], lambda pb=pb: nc.scalar.activation(out=xs[:, 0:255], in_=pb[0:64, 0:255], func=AF.Identity, bias=cb[:, 0:1], scale=1.0))
            kb.op("dve", [x2], [xs], lambda: nc.vector.tensor_tensor(out=x2[:, 0:255], in0=xs[:, 0:255], in1=xs[:, 0:255], op=ALU.mult))
            kb.op("dve", [x2], [x2], lambda: nc.vector.tensor_scalar(out=x2[:, 0:255], in0=x2[:, 0:255], scalar1=0.044715, scalar2=1.0, op0=ALU.mult, op1=ALU.add))
            kb.op("dve", [x2], [x2, xs], lambda: nc.vector.tensor_tensor(out=x2[:, 0:255], in0=x2[:, 0:255], in1=xs[:, 0:255], op=ALU.mult))
            kb.op("act", [sg], [x2], lambda: nc.scalar.activation(out=sg[:, 0:255], in_=x2[:, 0:255], func=AF.Sigmoid, scale=1.5957691216057308))
            kb.op("dve", [hT], [sg, xs], lambda: nc.vector.tensor_tensor(out=hT[:, 0:255], in0=sg[:, 0:255], in1=xs[:, 0:255], op=ALU.mult))
            if i == 0:
                pb = self.bank()
                kb.op("pe", [pb], [w2, hT], lambda pb=pb: nc.tensor.matmul(pb[0:64, 0:255], w2[:, :], hT[:, 0:255], start=True, stop=True))
                kb.op("act", [kcT], [pb], lambda pb=pb, g=g: nc.scalar.copy(out=kcT[:, g, 0:255], in_=pb[0:64, 0:255]))
            else:
                for ct in range(2):
                    m = 128 if ct == 0 else 127
                    pb = self.bank()
                    kb.op("pe", [pb], [w2, hT], lambda pb=pb, ct=ct, m=m: nc.tensor.matmul(pb[0:m, 0:64], hT[:, ct * 128:ct * 128 + m], w2[:, :], start=True, stop=True))
                    kb.op("act", [vc], [pb], lambda pb=pb, ct=ct, m=m, g=g: nc.scalar.copy(out=vc[0:m, ct, g, 0:64], in_=pb[0:m, 0:64]))
    if "kcT" in self.dbg:
        kb.dma("sp", self.scr["kcT"][:, :], kcT[:].rearrange("p a b -> p (a b)"), [], [kcT])
        kb.dma("sp", self.scr["vc"][:, :], vc[:].rearrange("p a b c -> p (a b c)"), [], [vc])
    self._nsa_main(l, dict(tab=tab, esel=esel, Wc=Wc, BT0=BT0, BT1=BT1, BT4=BT4, kcT=kcT, vc=vc))
    kb.pop()


Prog.p23_nsa = _nsa


def _nsa_main(self, l, R):
    kb, nc = self.kb, self.nc
    zT = self.scr["zT"]
    tab, esel, kcT, vc = R["tab"], R["esel"], R["kcT"], R["vc"]
    vstg = kb.sb("n_vstg", [128, NT, 4, 64], BF16)
    kb.dma("sp", vstg[:].rearrange("p i a d -> p (i a d)"), self.scr["vtm"][:, :], [vstg], [])
    gt = kb.sb("n_gt", [128, NT, 24], F32)
    kb.dma("sp", gt[:].rearrange("p i c -> p (i c)"), self.scr["ngate"][:, :], [gt], [])
    q4 = kb.sb("n_q4", [64, 4, S], BF16)
    ks = kb.sb("n_ks", [64, S], BF16)
    kw = kb.sb("n_kw", [64, S], BF16)
    va = kb.sb("n_va", [128, NT, 2, 65], BF16)
    sc = kb.sb("n_sc", [128, 4, 256], F32)
    pc = kb.sb("n_pc", [128, 4, 256], F32)
    mx = kb.sb("n_mx", [128, 4], F32)
    sm = kb.sb("n_sm", [128, 4], F32)
    rs = kb.sb("n_rs", [128, 4], F32)
    pacc = kb.sb("n_pacc", [128, 64, 4], F32)
    imp = kb.sb("n_imp", [128, 64], F32)
    imp2 = kb.sb("n_imp2", [128, 64], F32)
    m8a = kb.sb("n_m8a", [128, 8], F32)
    m8b = kb.sb("n_m8b", [128, 8], F32)
    selm = kb.sb("n_selm", [128, 64], F32)
    mrT = kb.sb("n_mrT", [64, 4, 128], BF16)
    pcT = kb.sb("n_pcT", [128, 4, 2, 128], BF16)
    coef = kb.sb("n_coef", [128, 3, 4], F32)
    oacc = kb.sb("n_oacc", [128, 4, 64], F32)
    otmp = kb.sb("n_otmp", [128, 4, 64], F32)
    oaT = kb.sb("n_oaT", [128, 2, 128], BF16)
    sTf = [kb.sb(f"n_sTf{i}", [128, 512], F32) for i in range(2)]
    pT = [kb.sb(f"n_pT{i}", [128, 512], BF16) for i in range(3)]
    pacc_f = pacc[:].rearrange("p j r -> p (j r)")
    brT0 = self.scr["brT0"].rearrange("(kc p) t -> p kc t", p=128)
    cnt = {"s": 0, "p": 0}
    pv_win, pv_sel = self.psb[5], self.psb[6]
    self.nrot = 5

    def exp_tile(pb, bias_t, eng_alt):
        p_t = pT[cnt["p"] % 3]
        cnt["p"] += 1
        if bias_t is not None:
            s_t = sTf[cnt["s"] % 2]
            cnt["s"] += 1
            kb.op("dve", [s_t], [pb, bias_t], lambda: nc.vector.scalar_tensor_tensor(
                out=s_t[:], in0=pb[:, :], scalar=0.125, in1=bias_t[:].rearrange("p a b -> p (a b)"), op0=ALU.mult, op1=ALU.add))
            kb.op("act", [p_t], [s_t], lambda: nc.scalar.activation(out=p_t[:], in_=s_t[:], func=AF.Exp))
        else:
            kb.op("act", [p_t], [pb], lambda: nc.scalar.activation(out=p_t[:], in_=pb[:, :], func=AF.Exp, scale=0.125))
        return p_t

    def finish_branch(pvb, br, jt, g, first):
        pv3 = pvb[:, 0:260].rearrange("p (h d) -> p h d", d=65)
        cf = coef[:, br, :]
        kb.op("dve", [coef], [pvb], lambda: nc.vector.tensor_scalar(out=cf, in0=pv3[:, :, 64], scalar1=1e-30, scalar2=None, op0=ALU.max))
        kb.op("dve", [coef], [coef], lambda: nc.vector.reciprocal(out=cf, in_=cf))
        gsl = gt[:, jt, g * 12:(g + 1) * 12].rearrange("p (h b) -> p h b", b=3)[:, :, br]
        kb.op("dve", [coef], [coef, gt], lambda: nc.vector.tensor_tensor(out=cf, in0=cf, in1=gsl, op=ALU.mult))
        cfb = cf.to_broadcast([128, 4, 64]) if False else coef[:, br, :, None].to_broadcast([128, 4, 64])
        if first:
            kb.op("dve", [oacc], [pvb, coef], lambda: nc.vector.tensor_tensor(out=oacc[:], in0=pv3[:, :, 0:64], in1=cfb, op=ALU.mult))
        else:
            kb.op("dve", [otmp], [pvb, coef], lambda: nc.vector.tensor_tensor(out=otmp[:], in0=pv3[:, :, 0:64], in1=cfb, op=ALU.mult))
            kb.op("pool", [oacc], [otmp, oacc], lambda: nc.gpsimd.tensor_tensor(out=oacc[:], in0=oacc[:], in1=otmp[:], op=ALU.add))

    for g in range(2):
        Wc, BT0, BT1, BT4 = R["Wc"][g], R["BT0"][g], R["BT1"][g], R["BT4"]
        kb.dma("pool", q4[:], zT[g * 256:(g + 1) * 256, :].rearrange("(h d) t -> d h t", d=64), [q4], [])
        kb.dma("pool", ks[:], zT[O_KV + 256 + g * 64:O_KV + 256 + g * 64 + 64, :], [ks], [])
        kb.dma("pool", kw[:], zT[O_KV + 512 + g * 64:O_KV + 512 + g * 64 + 64, :], [kw], [])
        kb.op("dve", [va], [], lambda: nc.vector.memset(va[:], 1.0))
        kb.op("pool", [va], [vstg], lambda g=g: nc.gpsimd.tensor_copy(out=va[:, :, 0, 0:64], in_=vstg[:, :, g, :]))
        kb.op("pool", [va], [vstg], lambda g=g: nc.gpsimd.tensor_copy(out=va[:, :, 1, 0:64], in_=vstg[:, :, 2 + g, :]))
        for jt in range(NT):
            qs = slice(jt * 128, (jt + 1) * 128)
            for half in range(2):
                pb = self.bank()
                def fsc(pb=pb, half=half):
                    for j in range(2):
                        ins = nc.tensor.matmul(pb[:, j * 256:(j + 1) * 256], q4[:, half * 2 + j, qs], kcT[:, g, :], start=True, stop=True)
                    return ins
                kb.op("pe", [pb], [q4, kcT], fsc, n=2)
                kb.op("dve", [sc], [pb, Wc], lambda pb=pb, half=half: nc.vector.scalar_tensor_tensor(
                    out=sc[:, half * 2:half * 2 + 2, :], in0=pb[:, :].rearrange("p (a b) -> p a b", a=2), scalar=0.125,
                    in1=Wc[:, half * 2:half * 2 + 2, 248 - 8 * jt:248 - 8 * jt + 256], op0=ALU.mult, op1=ALU.add))
            kb.op("dve", [mx], [sc], lambda: nc.vector.tensor_reduce(out=mx[:], in_=sc[:], axis=AX.X, op=ALU.max))
            kb.op("dve", [mx], [mx], lambda: nc.vector.tensor_scalar(out=mx[:], in0=mx[:], scalar1=-20000.0, scalar2=-1.0, op0=ALU.max, op1=ALU.mult))
            kb.op("dve", [sm], [], lambda: nc.vector.memset(sm[:], 0.0))
            for hg in range(4):
                kb.op("act", [pc, sm], [sc, mx], lambda hg=hg: nc.scalar.activation(out=pc[:, hg, :], in_=sc[:, hg, :], func=AF.Exp,
                                                                               bias=mx[:, hg:hg + 1], scale=1.0, accum_out=sm[:, hg:hg + 1]))
            kb.op("dve", [rs], [sm], lambda: nc.vector.tensor_scalar(out=rs[:], in0=sm[:], scalar1=1e-30, scalar2=None, op0=ALU.max))
            kb.op("dve", [rs], [rs], lambda: nc.vector.reciprocal(out=rs[:], in_=rs[:]))
            kb.op("dve", [pacc], [pc, rs], lambda: nc.vector.tensor_scalar(out=pacc_f, in0=pc[:, 0, :], scalar1=rs[:, 0:1], scalar2=None, op0=ALU.mult))
            for hg in range(1, 4):
                kb.op("dve", [pacc], [pc, rs, pacc], lambda hg=hg: nc.vector.scalar_tensor_tensor(
                    out=pacc_f, in0=pc[:, hg, :], scalar=rs[:, hg:hg + 1], in1=pacc_f, op0=ALU.mult, op1=ALU.add))
            kb.op("dve", [imp], [pacc], lambda: nc.vector.tensor_reduce(out=imp[:], in_=pacc[:], axis=AX.X, op=ALU.add))
            kb.op("dve", [imp], [imp, pacc], lambda: nc.vector.tensor_tensor(out=imp[:, 1:64], in0=imp[:, 1:64], in1=pacc[:, 0:63, 3], op=ALU.add))
            kb.op("dve", [imp], [imp, tab], lambda: nc.vector.tensor_tensor(out=imp[:], in0=imp[:], in1=tab[:, 888 + 62 - 2 * jt:888 + 62 - 2 * jt + 64], op=ALU.add))
            kb.op("dve", [imp], [imp], lambda: nc.vector.memset(imp[:, 0:1], 3.0e4))
            kb.op("dve", [m8a], [imp], lambda: nc.vector.max(out=m8a[:], in_=imp[:]))
            kb.op("dve", [imp2], [m8a, imp], lambda: nc.vector.match_replace(out=imp2[:], in_to_replace=m8a[:], in_values=imp[:], imm_value=-1.0e9))
            kb.op("dve", [m8b], [imp2], lambda: nc.vector.max(out=m8b[:], in_=imp2[:]))
            kb.op("dve", [selm], [imp, m8b], lambda: nc.vector.tensor_scalar(out=selm[:], in0=imp[:], scalar1=m8b[:, 7:8], scalar2=-240000.0, op0=ALU.is_lt, op1=ALU.mult))
            pb = self.bank()
            kb.op("pe", [pb], [selm, self.ident], lambda pb=pb: nc.tensor.transpose(out=pb[0:64, 0:128], in_=selm[:, :], identity=self.ident[:, :]))
            for hg in range(4):
                if hg % 2 == 0:
                    kb.op("act", [mrT], [pb], lambda pb=pb, hg=hg: nc.scalar.copy(out=mrT[:, hg, :], in_=pb[0:64, 0:128]))
                else:
                    kb.op("dve", [mrT], [pb], lambda pb=pb, hg=hg: nc.vector.tensor_copy(out=mrT[:, hg, :], in_=pb[0:64, 0:128]))
            nct = 2 if jt >= 16 else 1
            for half in range(2):
                pb = self.bank()
                def ftr(pb=pb, half=half):
                    for j in range(2):
                        for ct in range(nct):
                            ins = nc.tensor.transpose(out=pb[:, (j * 2 + ct) * 128:(j * 2 + ct + 1) * 128], in_=pc[:, half * 2 + j, ct * 128:(ct + 1) * 128], identity=self.ident[:, :])
                    return ins
                kb.op("pe", [pb], [pc, self.ident], ftr, n=2 * nct)
                src = pb[:, :].rearrange("p (j c q) -> p j c q", j=2, c=2)[:, :, 0:nct, :]
                kb.op("act" if half == 0 else "dve", [pcT], [pb],
                      (lambda src=src, half=half: nc.scalar.copy(out=pcT[:, half * 2:half * 2 + 2, 0:nct, :], in_=src)) if half == 0 else
                      (lambda src=src, half=half: nc.vector.tensor_copy(out=pcT[:, half * 2:half * 2 + 2, 0:nct, :], in_=src)))
            pvc = self.bank()
            def fpvc(pvc=pvc):
                for hg in range(4):
                    for ct in range(nct):
                        ins = nc.tensor.matmul(pvc[:, hg * 65:(hg + 1) * 65], pcT[:, hg, ct, :], vc[:, ct, g, :], start=(ct == 0 and hg == 0), stop=(ct == nct - 1 and hg == 3))
                return ins
            kb.op("pe", [pvc], [pcT, vc], fpvc, n=4 * nct)
            finish_branch(pvc, 0, jt, g, True)
            kts = [kt for kt in range(jt - 4, jt + 1) if kt >= 0]
            for ii, kt in enumerate(kts):
                dl = jt - kt
                pb = self.bank()
                kb.op("pe", [pb], [kw, q4], lambda pb=pb, kt=kt: nc.tensor.matmul(pb[:, :], kw[:, kt * 128:(kt + 1) * 128], q4[:, :, qs], start=True, stop=True))
                p_t = exp_tile(pb, {0: BT0, 1: BT1, 4: BT4}.get(dl), ii)
                def fpv(p_t=p_t, kt=kt, ii=ii):
                    for hg in range(4):
                        ins = nc.tensor.matmul(pv_win[:, hg * 65:(hg + 1) * 65], p_t[:, hg * 128:(hg + 1) * 128], va[:, kt, 1, :], start=(ii == 0 and hg == 0), stop=(ii == len(kts) - 1 and hg == 3))
                    return ins
                kb.op("pe", [pv_win], [p_t, va], fpv, n=4)
            finish_branch(pv_win, 2, jt, g, False)
            for kt in range(jt + 1):
                dl = jt - kt
                pb = self.bank()
                def fss(pb=pb, kt=kt):
                    nc.tensor.matmul(pb[:, :], ks[:, kt * 128:(kt + 1) * 128], q4[:, :, qs], start=True, stop=False)
                    return nc.tensor.matmul(pb[:, :], esel[:, kt, :], mrT[:, :, :], start=False, stop=True)
                kb.op("pe", [pb], [ks, q4, esel, mrT], fss, n=2)
                p_t = exp_tile(pb, {0: BT0, 1: BT1}.get(dl), kt)
                def fpv2(p_t=p_t, kt=kt):
                    for hg in range(4):
                        ins = nc.tensor.matmul(pv_sel[:, hg * 65:(hg + 1) * 65], p_t[:, hg * 128:(hg + 1) * 128], va[:, kt, 0, :], start=(kt == 0 and hg == 0), stop=(kt == jt and hg == 3))
                    return ins
                kb.op("pe", [pv_sel], [p_t, va], fpv2, n=4)
            finish_branch(pv_sel, 1, jt, g, False)
            pb = self.bank()
            oflat = oacc[:].rearrange("p h d -> p (h d)")
            def fto(pb=pb):
                for j in range(2):
                    ins = nc.tensor.transpose(out=pb[:, j * 128:(j + 1) * 128], in_=oflat[:, j * 128:(j + 1) * 128], identity=self.ident[:, :])
                return ins
            kb.op("pe", [pb], [oacc, self.ident], fto, n=2)
            kb.op("act", [oaT], [pb], lambda pb=pb: nc.scalar.copy(out=oaT[:], in_=pb[:, 0:256].rearrange("p (j q) -> p j q", j=2)))
            kb.dma("sp", brT0[:, g * 2:g * 2 + 2, qs], oaT[:], [], [oaT])
    self.nrot = 7


Prog._nsa_main = _nsa_main


def _conv(self, l):
    kb, nc = self.kb, self.nc
    zT = self.scr["zT"]
    brT1 = self.scr["brT1"]
    kb.push()
    prm = kb.sb("cv_prm", [34, 512], F32)
    kb.dma("sp", prm[0:31, :], self.din["conv_w"][l], [prm], [])
    kb.dma("sp", prm[31:32, :], self.din["conv_b"][l:l + 1, :], [prm], [])
    kb.dma("sp", prm[32:33, :], self.din["conv_norm_g"][l:l + 1, :], [prm], [])
    kb.dma("sp", prm[33:34, :], self.din["conv_norm_b"][l:l + 1, :], [prm], [])
    cw = kb.sb("cv_w", [128, 4, 34], F32)
    pb = self.bank()
    def ft(pb=pb):
        for cc in range(4):
            ins = nc.tensor.transpose(out=pb[:, cc * 34:(cc + 1) * 34], in_=prm[:, cc * 128:(cc + 1) * 128], identity=self.ident[0:34, 0:34])
        return ins
    kb.op("pe", [pb], [prm, self.ident], ft, n=4)
    kb.op("dve", [cw], [pb], lambda: nc.vector.tensor_copy(out=cw[:], in_=pb[:, 0:136].rearrange("p (c k) -> p c k", c=4)))
    y4 = kb.sb("cv_y", [128, 4, S], F32)
    a_t = kb.sb("cv_a", [128, S], F32)
    g_t = kb.sb("cv_g", [128, S], F32)
    u = kb.sb("cv_u", [128, 30 + S], F32)
    kb.op("dve", [u], [], lambda: nc.vector.memset(u[:, 0:30], 0.0))
    for cc in range(4):
        kb.dma("sp", a_t[:], zT[O_CONV + cc * 128:O_CONV + (cc + 1) * 128, :], [a_t], [])
        kb.dma("sp", g_t[:], zT[O_CONV + 512 + cc * 128:O_CONV + 512 + (cc + 1) * 128, :], [g_t], [])
        kb.op("act", [g_t], [g_t], lambda: nc.scalar.activation(out=g_t[:], in_=g_t[:], func=AF.Sigmoid))
        kb.op("pool", [u], [a_t, g_t], lambda: nc.gpsimd.tensor_tensor(out=u[:, 30:30 + S], in0=a_t[:], in1=g_t[:], op=ALU.mult))
        kb.op("dve", [y4], [u, cw], lambda cc=cc: nc.vector.tensor_scalar(out=y4[:, cc, :], in0=u[:, 0:S], scalar1=cw[:, cc, 0:1], scalar2=cw[:, cc, 31:32], op0=ALU.mult, op1=ALU.add))
        for k in range(1, 31):
            kb.op("dve", [y4], [u, cw, y4], lambda cc=cc, k=k: nc.vector.scalar_tensor_tensor(
                out=y4[:, cc, :], in0=u[:, k:k + S], scalar=cw[:, cc, k:k + 1], in1=y4[:, cc, :], op0=ALU.mult, op1=ALU.add))
    sq = kb.sb("cv_sq", [128, 4, 512], F32)
    mean = kb.sb("cv_mean", [128, 512], F32)
    msq = kb.sb("cv_msq", [128, 512], F32)
    rstd = kb.sb("cv_rstd", [128, 512], F32)
    tt = kb.sb("cv_tt", [128, 512], F32)
    ob = [kb.sb(f"cv_ob{i}", [128, 4, 512], BF16) for i in range(2)]
    brv = brT1.rearrange("(kc p) t -> p kc t", p=128)
    for tg in range(8):
        ts_ = slice(tg * 512, (tg + 1) * 512)
        kb.op("act", [sq], [y4], lambda: nc.scalar.activation(out=sq[:], in_=y4[:, :, ts_], func=AF.Square))
        p1, p2 = self.bank(), self.bank()
        def fs(p1=p1):
            for cc in range(4):
                ins = nc.tensor.matmul(p1[:, :], self.onesf[:, :], y4[:, cc, ts_], start=(cc == 0), stop=(cc == 3))
            return ins
        kb.op("pe", [p1], [y4, self.onesf], fs, n=4)
        def fq(p2=p2):
            for cc in range(4):
                ins = nc.tensor.matmul(p2[:, :], self.onesf[:, :], sq[:, cc, :], start=(cc == 0), stop=(cc == 3))
            return ins
        kb.op("pe", [p2], [sq, self.onesf], fq, n=4)
        kb.op("act", [mean], [p1], lambda p1=p1: nc.scalar.activation(out=mean[:], in_=p1[:, :], func=AF.Copy, scale=1.0 / 512))
        kb.op("dve", [msq], [mean], lambda: nc.vector.tensor_tensor(out=msq[:], in0=mean[:], in1=mean[:], op=ALU.mult))
        kb.op("dve", [rstd], [p2, msq], lambda p2=p2: nc.vector.scalar_tensor_tensor(out=rstd[:], in0=p2[:, :], scalar=1.0 / 512, in1=msq[:], op0=ALU.mult, op1=ALU.subtract))
        kb.op("act", [rstd], [rstd], lambda: nc.scalar.activation(out=rstd[:], in_=rstd[:], func=AF.Sqrt, bias=self.eps5[:, 0:1], scale=1.0))
        kb.op("dve", [rstd], [rstd], lambda: nc.vector.reciprocal(out=rstd[:], in_=rstd[:]))
        o_t = ob[tg % 2]
        for cc in range(4):
            kb.op("pool", [tt], [y4, mean], lambda cc=cc: nc.gpsimd.tensor_tensor(out=tt[:], in0=y4[:, cc, ts_], in1=mean[:], op=ALU.subtract))
            kb.op("dve", [tt], [tt, rstd], lambda: nc.vector.tensor_tensor(out=tt[:], in0=tt[:], in1=rstd[:], op=ALU.mult))
            kb.op("act", [o_t], [tt, cw], lambda cc=cc, o_t=o_t: nc.scalar.activation(out=o_t[:, cc, :], in_=tt[:], func=AF.Silu, scale=cw[:, cc, 32:33], bias=cw[:, cc, 33:34]))
        kb.dma("sp", brv[:, :, ts_], o_t[:], [], [o_t])
    kb.pop()


Prog.p4_conv = _conv


def _gla(self, l):
    kb, nc = self.kb, self.nc
    zT = self.scr["zT"]
    kb.push()
    qT = kb.sb("gl_qT", [64, 4, S], BF16)
    kT = kb.sb("gl_kT", [64, 4, S], BF16)
    ebl = kb.sb("gl_ebl", [64, 4, NT], F32)
    gw = kb.sb("gl_gw", [16, 256], F32)
    kb.dma("sp", gw[:], self.din["gla_gate_w"][l], [gw], [])
    gb = kb.sb("gl_gb", [64, 4], F32)
    kb.dma("sp", gb[:], self.din["gla_gate_b"][l].rearrange("(h d) -> d h", d=64), [gb], [], allow_slow_non_contiguous=True)
    kb.op("dve", [gb], [gb], lambda: nc.vector.tensor_scalar(out=gb[:], in0=gb[:], scalar1=-1.0, scalar2=None, op0=ALU.mult))
    gng = kb.sb("gl_gng", [128, 1], F32)
    kb.dma("sp", gng[:], self.din["gla_norm_g"][l].rearrange("(p o) -> p o", o=1), [gng], [])
    kb.push()
    ga = kb.sb("gl_ga", [16, S], F32)
    kb.dma("sp", ga[:], zT[O_GA:O_GA + 16, :], [ga], [])
    rmask = kb.sb("gl_rm", [64, S], F32)
    kb.op("pool", [rmask], [], lambda: nc.gpsimd.memset(rmask[:], 1.0))
    kb.op("pool", [rmask], [], lambda: nc.gpsimd.memset(rmask[:].rearrange("p (n c) -> p n c", c=128)[:, :, 0:1], 0.0))
    la = kb.sb("gl_la", [64, S], F32)
    cs = kb.sb("gl_cs", [64, S], F32)
    eb = kb.sb("gl_eb", [64, S], F32)
    st = kb.sb("gl_st", [64, S], F32)
    one1 = kb.sb("gl_one", [64, 1], F32)
    kb.op("dve", [one1], [], lambda: nc.vector.memset(one1[:], 1.0))
    for h in range(4):
        for tg in range(8):
            ts_ = slice(tg * 512, (tg + 1) * 512)
            pb = self.bank()
            kb.op("pe", [pb], [gw, ga], lambda pb=pb, ts_=ts_: nc.tensor.matmul(pb[0:64, :], gw[:, h * 64:(h + 1) * 64], ga[:, ts_], start=True, stop=True))
            kb.op("act", [la], [pb, gb], lambda pb=pb, ts_=ts_: nc.scalar.activation(out=la[:, ts_], in_=pb[0:64, :], func=AF.Exp, scale=-1.0, bias=gb[:, h:h + 1]))
        kb.op("act", [la], [la, one1], lambda: nc.scalar.activation(out=la[:], in_=la[:], func=AF.Ln, bias=one1[:, 0:1], scale=1.0))
        kb.op("dve", [cs], [la, rmask], lambda: nc.vector.tensor_tensor_scan(out=cs[:], data0=rmask[:], data1=la[:], initial=0.0, op0=ALU.mult, op1=ALU.add))
        kb.op("act", [eb], [cs], lambda: nc.scalar.activation(out=eb[:], in_=cs[:], func=AF.Exp, scale=-1.0 / 16))
        kb.op("pool", [ebl], [eb], lambda: nc.gpsimd.tensor_copy(out=ebl[:, h, :], in_=eb[:].rearrange("p (n c) -> p n c", c=128)[:, :, 127]))
        kb.dma("sp", st[:], zT[O_GQ + h * 64:O_GQ + (h + 1) * 64, :], [st], [])
        kb.op("dve", [qT], [st, eb], lambda: nc.vector.scalar_tensor_tensor(out=qT[:, h, :], in0=st[:], scalar=0.125, in1=eb[:], op0=ALU.mult, op1=ALU.mult))
        kb.op("act", [eb], [cs], lambda: nc.scalar.activation(out=eb[:], in_=cs[:], func=AF.Exp, scale=1.0 / 16))
        kb.dma("sp", st[:], zT[O_GK + h * 64:O_GK + (h + 1) * 64, :], [st], [])
        kb.op("dve", [kT], [st, eb], lambda: nc.vector.tensor_tensor(out=kT[:, h, :], in0=st[:], in1=eb[:], op=ALU.mult))
    kb.pop()
    v = kb.sb("gl_v", [128, NT, 512], BF16)
    kb.dma("sp", v[:].rearrange("p i c -> p (i c)"), self.scr["gvtm"][:, :], [v], [])
    sgr = kb.sb("gl_sgr", [128, 4, S], BF16)
    stg = kb.sb("gl_stg", [128, S], F32)
    for c in range(4):
        kb.dma("sp", stg[:], zT[O_GR + c * 128:O_GR + (c + 1) * 128, :], [stg], [])
        kb.op("act", [sgr], [stg], lambda c=c: nc.scalar.activation(out=sgr[:, c, :], in_=stg[:], func=AF.Silu))
    ocT = kb.sb("gl_ocT", [128, 4, S], BF16)
    cmask = kb.sb("gl_cmask", [128, 4, 128], F32)
    kb.op("pool", [cmask], [], lambda: nc.gpsimd.memset(cmask[:], 1.0))
    for h in range(4):
        kb.op("pool", [cmask], [cmask], lambda h=h: nc.gpsimd.affine_select(out=cmask[:, h, :], in_=cmask[:, h, :], pattern=[[1, 128]], compare_op=ALU.is_ge, fill=0.0, base=0, channel_multiplier=-1))
    S4 = kb.sb("gl_S4", [64, 4, 128], F32)
    S4b = [kb.sb(f"gl_S4b{i}", [64, 4, 128], BF16) for i in range(2)]
    tmpS = kb.sb("gl_tmpS", [64, 4, 128], F32)
    kb.op("dve", [S4], [], lambda: nc.vector.memset(S4[:], 0.0))
    kb.op("dve", [S4b[0]], [], lambda: nc.vector.memset(S4b[0][:], 0.0))
    attn = [kb.sb(f"gl_attn{i}", [128, 4, 128], BF16) for i in range(2)]
    ktm = [kb.sb(f"gl_ktm{i}", [128, 4, 64], BF16) for i in range(2)]
    sqo = kb.sb("gl_sqo", [128, 512], F32)
    rinv = kb.sb("gl_rinv", [128, 512], F32)
    on = kb.sb("gl_on", [128, 512], F32)
    for i in range(NT):
        tsl = slice(i * 128, (i + 1) * 128)
        Sb = S4b[i % 2]
        Sb_next = S4b[(i + 1) % 2]
        at = attn[i % 2]
        kt_ = ktm[i % 2]
        pb = self.bank()
        def fsc(pb=pb):
            for h in range(4):
                ins = nc.tensor.matmul(pb[:, h * 128:(h + 1) * 128], kT[:, h, tsl], qT[:, h, tsl], start=True, stop=True)
            return ins
        kb.op("pe", [pb], [kT, qT], fsc, n=4)
        kb.op("dve", [at], [pb, cmask], lambda pb=pb, at=at: nc.vector.tensor_tensor(out=at[:].rearrange("p a b -> p (a b)"), in0=pb[:, :], in1=cmask[:].rearrange("p a b -> p (a b)"), op=ALU.mult))
        pt = self.ps_bf
        def ftr(pt=pt):
            for h in range(4):
                ins = nc.tensor.transpose(out=pt[:, h * 64:(h + 1) * 64], in_=kT[:, h, tsl], identity=self.identb[0:64, 0:64])
            return ins
        kb.op("pe", [pt], [kT, self.identb], ftr, n=4)
        kb.op("act", [kt_], [pt], lambda pt=pt, kt_=kt_: nc.scalar.copy(out=kt_[:].rearrange("p a b -> p (a b)"), in_=pt[:, 0:256]))
        po = self.bank()
        def fo(po=po, at=at, Sb=Sb):
            for h in range(4):
                nc.tensor.matmul(po[:, h * 128:(h + 1) * 128], v[:, i, h * 128:(h + 1) * 128], at[:, h, :], start=(h == 0), stop=False)
            for h in range(4):
                ins = nc.tensor.matmul(po[:, h * 128:(h + 1) * 128], Sb[:, h, :], qT[:, h, tsl], start=False, stop=(h == 3))
            return ins
        kb.op("pe", [po], [v, at, Sb, qT], fo, n=8)
        pd = self.bank()
        def fd(pd=pd, kt_=kt_):
            for h in range(4):
                ins = nc.tensor.matmul(pd[0:64, h * 128:(h + 1) * 128], kt_[:, h, :], v[:, i, h * 128:(h + 1) * 128], start=(h == 0), stop=(h == 3))
            return ins
        kb.op("pe", [pd], [kt_, v], fd, n=4)
        kb.op("dve", [tmpS], [pd, S4], lambda pd=pd: nc.vector.tensor_tensor(out=tmpS[:], in0=pd[0:64, :].rearrange("p (h e) -> p h e", h=4), in1=S4[:], op=ALU.add))
        kb.op("dve", [S4], [tmpS, ebl], lambda: nc.vector.tensor_tensor(out=S4[:], in0=tmpS[:], in1=ebl[:, :, i:i + 1].to_broadcast([64, 4, 128]), op=ALU.mult))
        kb.op("act", [Sb_next], [S4], lambda Sb_next=Sb_next: nc.scalar.copy(out=Sb_next[:], in_=S4[:]))
        kb.op("act", [sqo], [po], lambda po=po: nc.scalar.activation(out=sqo[:], in_=po[:, :], func=AF.Square))
        pq = self.bank()
        kb.op("pe", [pq], [sqo, self.onesf], lambda pq=pq: nc.tensor.matmul(pq[:, :], self.onesf[:, :], sqo[:], start=True, stop=True))
        kb.op("act", [rinv], [pq], lambda pq=pq: nc.scalar.activation(out=rinv[:], in_=pq[:, :], func=AF.Sqrt, bias=self.eps6[:, 0:1], scale=1.0 / 128))
        kb.op("dve", [rinv], [rinv], lambda: nc.vector.reciprocal(out=rinv[:], in_=rinv[:]))
        kb.op("dve", [on], [po, rinv], lambda po=po: nc.vector.tensor_tensor(out=on[:], in0=po[:, :], in1=rinv[:], op=ALU.mult))
        kb.op("dve", [ocT], [on, gng, sgr], lambda: nc.vector.scalar_tensor_tensor(out=ocT[:, :, tsl], in0=on[:].rearrange("p (h t) -> p h t", h=4), scalar=gng[:, 0:1], in1=sgr[:, :, tsl], op0=ALU.mult, op1=ALU.mult))
    kb.dma("sp", self.scr["brT2"].rearrange("(kc p) t -> p kc t", p=128), ocT[:], [], [ocT])
    kb.pop()


Prog.p5_gla = _gla


def _ln_setup(self, l, idx):
    kb = self.kb
    lng = kb.sb("ln_g", [128, D], F32)
    lnb = kb.sb("ln_b", [128, D], F32)
    kb.dma("sp", lng[:], self.din["norm_g"][l, idx:idx + 1, :].to_broadcast([128, D]), [lng], [])
    kb.dma("sp", lnb[:], self.din["norm_b"][l, idx:idx + 1, :].to_broadcast([128, D]), [lnb], [])
    st = kb.sb("ln_st", [128, 2, 6], F32)
    mv = kb.sb("ln_mv", [128, 2], F32)
    return dict(g=lng, b=lnb, st=st, mv=mv)


def _ln_tile(self, L, h, o):
    kb, nc = self.kb, self.nc
    st, mv = L["st"], L["mv"]
    for j in range(2):
        kb.op("dve", [st], [h], lambda j=j: nc.vector.bn_stats(out=st[:, j, :], in_=h[:, j * 512:(j + 1) * 512]))
    kb.op("dve", [mv], [st], lambda: nc.vector.bn_aggr(out=mv[:], in_=st[:].rearrange("p a b -> p (a b)")))
    kb.op("act", [mv], [mv], lambda: nc.scalar.activation(out=mv[:, 1:2], in_=mv[:, 1:2], func=AF.Sqrt, bias=self.eps5[:, 0:1], scale=1.0))
    kb.op("dve", [mv], [mv], lambda: nc.vector.reciprocal(out=mv[:, 1:2], in_=mv[:, 1:2]))
    kb.op("dve", [h], [h, mv], lambda: nc.vector.tensor_scalar(out=h[:], in0=h[:], scalar1=mv[:, 0:1], scalar2=mv[:, 1:2], op0=ALU.subtract, op1=ALU.mult))
    kb.op("pool", [h], [h, L["g"]], lambda: nc.gpsimd.tensor_tensor(out=h[:], in0=h[:], in1=L["g"][:], op=ALU.mult))
    kb.op("pool", [o], [h, L["b"]], lambda: nc.gpsimd.tensor_tensor(out=o[:], in0=h[:], in1=L["b"][:], op=ALU.add))


Prog.ln_setup = _ln_setup
Prog.ln_tile = _ln_tile


def _proj_res_ln(self, L, aT, w, xsrc, xdst, tg, xin, hbuf, obuf, cnt):
    kb, nc = self.kb, self.nc
    for tt_ in range(4):
        row0 = tg * 512 + tt_ * 128
        x_t = xin[cnt[0] % 2]
        h_t = hbuf[cnt[0] % 2]
        o_t = obuf[cnt[0] % 2]
        cnt[0] += 1
        kb.dma("sp", x_t[:], xsrc[row0:row0 + 128, :], [x_t], [])
        for half in range(2):
            pb = self.bank()
            def f(pb=pb, half=half, tt_=tt_):
                for kc in range(8):
                    ins = nc.tensor.matmul(pb[:, :], aT[:, kc, tt_ * 128:(tt_ + 1) * 128], w[:, kc, half * 512:(half + 1) * 512], start=(kc == 0), stop=(kc == 7))
                return ins
            kb.op("pe", [pb], [aT, w], f, n=8)
            kb.op("dve", [h_t], [x_t, pb], lambda pb=pb, half=half, x_t=x_t, h_t=h_t: nc.vector.scalar_tensor_tensor(
                out=h_t[:, half * 512:(half + 1) * 512], in0=x_t[:, half * 512:(half + 1) * 512], scalar=ALPHA, in1=pb[:, :], op0=ALU.mult, op1=ALU.add))
        self.ln_tile(L, h_t, o_t)
        kb.dma("sp", xdst[row0:row0 + 128, :], o_t[:], [], [o_t])


Prog.proj_res_ln = _proj_res_ln


def _merge(self, l, xsrc, xdst):
    kb, nc = self.kb, self.nc
    zT = self.scr["zT"]
    kb.push()
    wbr = kb.sb("mg_wbr", [128, 3, 4, D], BF16)
    for br in range(3):
        kb.dma("pool", wbr[:, br, :, :], self.din["w_branch"][l, br].rearrange("(kc p) n -> p kc n", p=128), [wbr], [])
    wo = kb.sb("mg_wo", [128, 8, D], BF16)
    kb.dma("pool", wo[:], self.din["w_out"][l].rearrange("(kc p) n -> p kc n", p=128), [wo], [])
    L = self.ln_setup(l, 0)
    brs = kb.sb("mg_br", [128, 3, 4, 512], BF16)
    mg = kb.sb("mg_g", [128, 24, 512], F32)
    mT = kb.sb("mg_mT", [128, 8, 512], BF16)
    t1 = kb.sb("mg_t1", [128, 512], F32)
    t2 = kb.sb("mg_t2", [128, 512], F32)
    xin = [kb.sb(f"mg_x{i}", [128, D], F32) for i in range(2)]
    hb = [kb.sb(f"mg_h{i}", [128, D], F32) for i in range(2)]
    ob = [kb.sb(f"mg_o{i}", [128, D], F32) for i in range(2)]
    cnt = [0]
    mgv = zT[O_MG:O_MG + 3072, :].rearrange("(c p) t -> p c t", p=128)
    for tg in range(8):
        ts_ = slice(tg * 512, (tg + 1) * 512)
        for br in range(3):
            kb.dma("sp", brs[:, br, :, :], self.scr[f"brT{br}"].rearrange("(kc p) t -> p kc t", p=128)[:, :, ts_], [brs], [])
        for c3 in range(3):
            kb.dma("act", mg[:, c3 * 8:(c3 + 1) * 8, :], mgv[:, c3 * 8:(c3 + 1) * 8, ts_], [mg], [])
        kb.op("act", [mg], [mg], lambda: nc.scalar.activation(out=mg[:], in_=mg[:], func=AF.Sigmoid))
        for dm in range(8):
            pbs = []
            for br in range(3):
                pb = self.bank()
                def f(pb=pb, br=br, dm=dm):
                    for kc in range(4):
                        ins = nc.tensor.matmul(pb[:, :], wbr[:, br, kc, dm * 128:(dm + 1) * 128], brs[:, br, kc, :], start=(kc == 0), stop=(kc == 3))
                    return ins
                kb.op("pe", [pb], [wbr, brs], f, n=4)
                pbs.append(pb)
            kb.op("dve", [t1], [pbs[0], mg], lambda pbs=pbs, dm=dm: nc.vector.tensor_tensor(out=t1[:], in0=pbs[0][:, :], in1=mg[:, dm, :], op=ALU.mult))
            kb.op("dve", [t2], [pbs[1], mg], lambda pbs=pbs, dm=dm: nc.vector.tensor_tensor(out=t2[:], in0=pbs[1][:, :], in1=mg[:, 8 + dm, :], op=ALU.mult))
            kb.op("pool", [t1], [t1, t2], lambda: nc.gpsimd.tensor_tensor(out=t1[:], in0=t1[:], in1=t2[:], op=ALU.add))
            kb.op("dve", [t2], [pbs[2], mg], lambda pbs=pbs, dm=dm: nc.vector.tensor_tensor(out=t2[:], in0=pbs[2][:, :], in1=mg[:, 16 + dm, :], op=ALU.mult))
            kb.op("pool", [mT], [t1, t2], lambda dm=dm: nc.gpsimd.tensor_tensor(out=mT[:, dm, :], in0=t1[:], in1=t2[:], op=ALU.add))
        self.proj_res_ln(L, mT, wo, xsrc, xdst, tg, xin, hb, ob, cnt)
    kb.pop()


Prog.p6_merge = _merge


def _xattn(self, l, xsrc, xdst):
    kb, nc = self.kb, self.nc
    kb.push()
    wq = kb.sb("xa_wq", [128, 8, D], BF16)
    kb.dma("pool", wq[:], self.din["xa_wq"][l].rearrange("(kc p) n -> p kc n", p=128), [wq], [])
    wo = kb.sb("xa_wo", [128, 8, D], BF16)
    kb.dma("pool", wo[:], self.din["xa_wo"][l].rearrange("(kc p) n -> p kc n", p=128), [wo], [])
    L = self.ln_setup(l, 1)
    memT = kb.sb("xa_memT", [128, 8, MEM], BF16)
    self.build_xT(self.din["mem"], memT, ntiles=2)
    kTm = kb.sb("xa_kT", [128, 8, MEM], BF16)
    vm = kb.sb("xa_v", [128, 2, D], BF16)
    wkv = [kb.sb(f"xa_wkv{i}", [128, 8, 512], BF16) for i in range(2)]
    wkvv = self.din["xa_wkv"][l].rearrange("(kc p) n -> p kc n", p=128)
    for grp in range(4):
        w = wkv[grp % 2]
        kb.dma("pool", w[:], wkvv[:, :, grp * 512:(grp + 1) * 512], [w], [])
        if grp < 2:
            for cc in range(4):
                pb = self.bank()
                def f(pb=pb, w=w, cc=cc):
                    for kc in range(8):
                        ins = nc.tensor.matmul(pb[:, 0:MEM], w[:, kc, cc * 128:(cc + 1) * 128], memT[:, kc, :], start=(kc == 0), stop=(kc == 7))
                    return ins
                kb.op("pe", [pb], [w, memT], f, n=8)
                kb.op("act", [kTm], [pb], lambda pb=pb, j=grp * 4 + cc: nc.scalar.copy(out=kTm[:, j, :], in_=pb[:, 0:MEM]))
        else:
            for mt in range(2):
                pb = self.bank()
                def f(pb=pb, w=w, mt=mt):
                    for kc in range(8):
                        ins = nc.tensor.matmul(pb[:, :], memT[:, kc, mt * 128:(mt + 1) * 128], w[:, kc, :], start=(kc == 0), stop=(kc == 7))
                    return ins
                kb.op("pe", [pb], [w, memT], f, n=8)
                kb.op("act", [vm], [pb], lambda pb=pb, mt=mt, c0=(grp - 2) * 512: nc.scalar.copy(out=vm[:, mt, c0:c0 + 512], in_=pb[:, :]))
    xT_t = kb.sb("xa_xT", [128, 8, 512], BF16)
    qT = kb.sb("xa_qT", [128, 8, 512], BF16)
    oT = kb.sb("xa_oT", [128, 8, 512], BF16)
    pTm = [kb.sb(f"xa_pT{i}", [128, 512], BF16) for i in range(4)]
    rec = kb.sb("xa_rec", [128, 512], F32)
    xin = [kb.sb(f"xa_x{i}", [128, D], F32) for i in range(2)]
    hb = [kb.sb(f"xa_h{i}", [128, D], F32) for i in range(2)]
    ob = [kb.sb(f"xa_o{i}", [128, D], F32) for i in range(2)]
    cnt = [0]
    stg = [kb.sb(f"xa_stg{i}", [128, D], F32) for i in range(2)]
    for tg in range(8):
        self.build_xT(xsrc[tg * 512:(tg + 1) * 512, :], xT_t, ntiles=4, stg=stg)
        for j in range(8):
            pb = self.bank()
            def f(pb=pb, j=j):
                for kc in range(8):
                    ins = nc.tensor.matmul(pb[:, :], wq[:, kc, j * 128:(j + 1) * 128], xT_t[:, kc, :], start=(kc == 0), stop=(kc == 7))
                return ins
            kb.op("pe", [pb], [wq, xT_t], f, n=8)
            if j % 2 == 0:
                kb.op("act", [qT], [pb], lambda pb=pb, j=j: nc.scalar.activation(out=qT[:, j, :], in_=pb[:, :], func=AF.Copy, scale=1.0 / 16))
            else:
                kb.op("dve", [qT], [pb], lambda pb=pb, j=j: nc.vector.tensor_scalar(out=qT[:, j, :], in0=pb[:, :], scalar1=1.0 / 16, scalar2=None, op0=ALU.mult))
        for h in range(4):
            pts = []
            for mt in range(2):
                pb = self.bank()
                def f(pb=pb, h=h, mt=mt):
                    for c in range(2):
                        ins = nc.tensor.matmul(pb[:, :], kTm[:, 2 * h + c, mt * 128:(mt + 1) * 128], qT[:, 2 * h + c, :], start=(c == 0), stop=(c == 1))
                    return ins
                kb.op("pe", [pb], [kTm, qT], f, n=2)
                p_t = pTm[(h % 2) * 2 + mt]
                kb.op("act", [p_t], [pb], lambda pb=pb, p_t=p_t: nc.scalar.activation(out=p_t[:], in_=pb[:, :], func=AF.Exp))
                pts.append(p_t)
            pb = self.bank()
            def fs(pb=pb, pts=pts):
                for mt in range(2):
                    ins = nc.tensor.matmul(pb[:, :], self.onesb[:, :], pts[mt][:], start=(mt == 0), stop=(mt == 1))
                return ins
            kb.op("pe", [pb], [self.onesb] + pts, fs, n=2)
            kb.op("dve", [rec], [pb], lambda pb=pb: nc.vector.reciprocal(out=rec[:], in_=pb[:, :]))
            for c in range(2):
                pb = self.bank()
                def fo(pb=pb, pts=pts, h=h, c=c):
                    for mt in range(2):
                        ins = nc.tensor.matmul(pb[:, :], vm[:, mt, h * 256 + c * 128:h * 256 + (c + 1) * 128], pts[mt][:], start=(mt == 0), stop=(mt == 1))
                    return ins
                kb.op("pe", [pb], [vm] + pts, fo, n=2)
                kb.op("dve", [oT], [pb, rec], lambda pb=pb, h=h, c=c: nc.vector.tensor_tensor(out=oT[:, 2 * h + c, :], in0=pb[:, :], in1=rec[:], op=ALU.mult))
        self.proj_res_ln(L, oT, wo, xsrc, xdst, tg, xin, hb, ob, cnt)
    kb.pop()


Prog.p7_xattn = _xattn


def _moe(self, l, xsrc, xdst):
    kb, nc = self.kb, self.nc
    xs, ys = self.scr["moe_xs"], self.scr["moe_ys"]
    kb.push()
    didx = kb.sb("mo_didx", [128, NT, 4], I32)
    gts = kb.sb("mo_gts", [128, NT, 4], F32)
    kb.push()
    zt = kb.sb("mo_zero", [128, 8192], BF16)
    kb.op("pool", [zt], [], lambda: nc.gpsimd.memset(zt[:], 0.0))
    xsz = xs.rearrange("(a p r) d -> a p (r d)", p=128, r=8)
    for a in range(NE * CAP // 1024):
        kb.dma("sp" if a % 2 == 0 else "act", xsz[a], zt[:], [], [zt])
    kb.barrier()
    rw = kb.sb("mo_rw", [128, 8, NE], F32)
    kb.dma("sp", rw[:], self.din["router_w"][l].rearrange("(kc p) e -> p kc e", p=128), [rw], [])
    rb = kb.sb("mo_rb", [128, NE], F32)
    kb.dma("sp", rb[:], self.din["router_b"][l:l + 1, :].to_broadcast([128, NE]), [rb], [])
    eoff = kb.sb("mo_eoff", [128, NE], F32)
    for e in range(NE):
        kb.op("pool", [eoff], [], lambda e=e: nc.gpsimd.memset(eoff[:, e:e + 1], float(e * CAP)))
    lst = kb.sb("mo_lst", [128, 128], BF16)
    kb.op("pool", [lst], [], lambda: nc.gpsimd.memset(lst[:], 1.0))
    kb.op("pool", [lst], [lst], lambda: nc.gpsimd.affine_select(out=lst[:], in_=lst[:], pattern=[[1, 128]], compare_op=ALU.is_gt, fill=0.0, base=0, channel_multiplier=-1))
    off = kb.sb("mo_off", [128, NE], F32)
    kb.op("dve", [off], [], lambda: nc.vector.memset(off[:], 0.0))
    x_t = [kb.sb(f"mo_x{i}", [128, D], F32) for i in range(2)]
    xb = [kb.sb(f"mo_xb{i}", [128, D], BF16) for i in range(2)]
    xTf = kb.sb("mo_xTf", [128, 8, 128], F32)
    lg = kb.sb("mo_lg", [128, NE], F32)
    m8 = kb.sb("mo_m8", [128, 8], F32)
    msk = kb.sb("mo_msk", [128, NE], BF16)
    dest = kb.sb("mo_dest", [128, NE], F32)
    oh = kb.sb("mo_oh", [128, NE], F32)
    dk = kb.sb("mo_dk", [128, 4], F32)
    nm0 = kb.sb("mo_nm0", [128, 1], F32)
    e4 = kb.sb("mo_e4", [128, 4], F32)
    s4 = kb.sb("mo_s4", [128, 1], F32)
    for i in range(NT):
        xt, xbt = x_t[i % 2], xb[i % 2]
        kb.dma("sp", xt[:], xsrc[i * 128:(i + 1) * 128, :], [xt], [])
        kb.op("act", [xbt], [xt], lambda xt=xt, xbt=xbt: nc.scalar.copy(out=xbt[:], in_=xt[:]))
        for half in range(2):
            pb = self.bank()
            def f(pb=pb, xt=xt, half=half):
                for j in range(4):
                    kc = half * 4 + j
                    ins = nc.tensor.transpose(out=pb[:, j * 128:(j + 1) * 128], in_=xt[:, kc * 128:(kc + 1) * 128], identity=self.ident[:])
                return ins
            kb.op("pe", [pb], [xt, self.ident], f, n=4)
            kb.op("dve", [xTf], [pb], lambda pb=pb, half=half: nc.vector.tensor_copy(out=xTf[:, half * 4:half * 4 + 4, :], in_=pb[:].rearrange("p (j t) -> p j t", j=4)))
        pb = self.bank()
        def fr(pb=pb):
            for kc in range(8):
                ins = nc.tensor.matmul(pb[:, 0:NE], xTf[:, kc, :], rw[:, kc, :], start=(kc == 0), stop=(kc == 7))
            return ins
        kb.op("pe", [pb], [xTf, rw], fr, n=8)
        kb.op("dve", [lg], [pb, rb], lambda pb=pb: nc.vector.tensor_tensor(out=lg[:], in0=pb[:, 0:NE], in1=rb[:], op=ALU.add))
        kb.op("dve", [m8], [lg], lambda: nc.vector.max(out=m8[:], in_=lg[:]))
        kb.op("dve", [msk], [lg, m8], lambda: nc.vector.tensor_scalar(out=msk[:], in0=lg[:], scalar1=m8[:, 3:4], scalar2=None, op0=ALU.is_ge))
        pp = self.bank()
        kb.op("pe", [pp], [lst, msk], lambda pp=pp: nc.tensor.matmul(pp[:, 0:NE], lst[:, :], msk[:, :], start=True, stop=True))
        kb.op("dve", [dest], [pp, off], lambda pp=pp: nc.vector.tensor_tensor(out=dest[:], in0=pp[:, 0:NE], in1=off[:], op=ALU.add))
        kb.op("dve", [dest], [dest, eoff], lambda: nc.vector.scalar_tensor_tensor(out=dest[:], in0=dest[:], scalar=float(CAP - 1), in1=eoff[:], op0=ALU.min, op1=ALU.add))
        pc_ = self.bank()
        kb.op("pe", [pc_], [self.onesb, msk], lambda pc_=pc_: nc.tensor.matmul(pc_[:, 0:NE], self.onesb[:, :], msk[:, :], start=True, stop=True))
        kb.op("dve", [off], [off, pc_], lambda pc_=pc_: nc.vector.tensor_tensor(out=off[:], in0=off[:], in1=pc_[:, 0:NE], op=ALU.add))
        kb.op("dve", [nm0], [m8], lambda: nc.vector.tensor_scalar(out=nm0[:], in0=m8[:, 0:1], scalar1=-1.0, scalar2=None, op0=ALU.mult))
        kb.op("dve", [s4], [], lambda: nc.vector.memset(s4[:], 0.0))
        kb.op("act", [e4, s4], [m8, nm0], lambda: nc.scalar.activation(out=e4[:], in_=m8[:, 0:4], func=AF.Exp, bias=nm0[:, 0:1], scale=1.0, accum_out=s4[:, 0:1]))
        kb.op("dve", [s4], [s4], lambda: nc.vector.reciprocal(out=s4[:], in_=s4[:]))
        kb.op("dve", [gts], [e4, s4], lambda i=i: nc.vector.tensor_scalar(out=gts[:, i, :], in0=e4[:], scalar1=s4[:, 0:1], scalar2=None, op0=ALU.mult))
        for k in range(4):
            kb.op("dve", [oh], [lg, m8, dest], lambda k=k: nc.vector.scalar_tensor_tensor(out=oh[:], in0=lg[:], scalar=m8[:, k:k + 1], in1=dest[:], op0=ALU.is_equal, op1=ALU.mult))
            kb.op("dve", [dk], [oh], lambda k=k: nc.vector.tensor_reduce(out=dk[:, k:k + 1], in_=oh[:], axis=AX.X, op=ALU.add))
        kb.op("dve", [didx], [dk], lambda i=i: nc.vector.tensor_copy(out=didx[:, i, :], in_=dk[:]))
        for k in range(4):
            kb.idma(xs[:, :], bass.IndirectOffsetOnAxis(ap=didx[:, i, k:k + 1], axis=0), xbt[:, :], None, [], [xbt, didx])
    kb.pop()
    kb.push()
    bgr = kb.sb("mo_bgr", [NE, 2 * D], F32)
    kb.dma("sp", bgr[:], self.din["expert_b_gu"][l], [bgr], [])
    bgu = kb.sb("mo_bgu", [128, 16, NE], F32)
    for half in range(2):
        pb = self.bank()
        def fb(pb=pb, half=half):
            for j in range(8):
                c = half * 8 + j
                ins = nc.tensor.transpose(out=pb[:, j * NE:(j + 1) * NE], in_=bgr[:, c * 128:(c + 1) * 128], identity=self.ident[0:NE, 0:NE])
            return ins
        kb.op("pe", [pb], [bgr, self.ident], fb, n=8)
        kb.op("dve", [bgu], [pb], lambda pb=pb, half=half: nc.vector.tensor_copy(out=bgu[:, half * 8:half * 8 + 8, :], in_=pb[:, 0:8 * NE].rearrange("p (c e) -> p c e", c=8)))
    wgu = [kb.sb(f"mo_wgu{i}", [128, 8, 2 * D], BF16) for i in range(2)]
    wdn = [kb.sb(f"mo_wdn{i}", [128, 8, D], BF16) for i in range(2)]
    bdn = [kb.sb(f"mo_bdn{i}", [128, D], F32) for i in range(2)]
    xse = [kb.sb(f"mo_xse{i}", [128, D], BF16) for i in range(2)]
    xsT = kb.sb("mo_xsT", [128, 8, CAP], BF16)
    hT = kb.sb("mo_hT", [128, 8, CAP], BF16)
    gp = kb.sb("mo_gp", [128, 512], F32)
    sgm = kb.sb("mo_sgm", [128, 512], F32)
    up = kb.sb("mo_up", [128, 512], F32)
    yst = [kb.sb(f"mo_ys{i}", [128, D], F32) for i in range(2)]
    yc = 0
    for e in range(NE):
        wg, wd, bd = wgu[e % 2], wdn[e % 2], bdn[e % 2]
        wgv = self.din["expert_w_gu"][l, e].rearrange("(kc p) n -> p kc n", p=128)
        kb.dma("pool", wg[:, 0:4, :], wgv[:, 0:4, :], [wg], [])
        kb.dma("pool", wg[:, 4:8, :], wgv[:, 4:8, :], [wg], [])
        kb.dma("pool", wd[:], self.din["expert_w_down"][l, e].rearrange("(kc p) n -> p kc n", p=128), [wd], [])
        kb.dma("sp", bd[:], self.din["expert_b_down"][l, e:e + 1, :].to_broadcast([128, D]), [bd], [])
        for st in range(CAP // 128):
            xt = xse[st % 2]
            kb.dma("sp", xt[:], xs[e * CAP + st * 128:e * CAP + (st + 1) * 128, :], [xt], [])
            pt = self.ps_bf
            def ft(pt=pt, xt=xt):
                for kc in range(8):
                    ins = nc.tensor.transpose(out=pt[:, kc * 128:(kc + 1) * 128], in_=xt[:, kc * 128:(kc + 1) * 128], identity=self.identb[:, :])
                return ins
            kb.op("pe", [pt], [xt, self.identb], ft, n=8)
            if st % 2 == 0:
                kb.op("act", [xsT], [pt], lambda pt=pt, st=st: nc.scalar.copy(out=xsT[:, :, st * 128:(st + 1) * 128], in_=pt[:, :].rearrange("p (k t) -> p k t", k=8)))
            else:
                kb.op("dve", [xsT], [pt], lambda pt=pt, st=st: nc.vector.tensor_copy(out=xsT[:, :, st * 128:(st + 1) * 128], in_=pt[:, :].rearrange("p (k t) -> p k t", k=8)))
        for c in range(8):
            for (s0, sn) in ((0, 512), (512, CAP - 512)):
                pg, pu = self.bank(), self.bank()
                def fg(pg=pg, wg=wg, c=c, s0=s0, sn=sn):
                    for kc in range(8):
                        ins = nc.tensor.matmul(pg[:, 0:sn], wg[:, kc, c * 128:(c + 1) * 128], xsT[:, kc, s0:s0 + sn], start=(kc == 0), stop=(kc == 7))
                    return ins
                kb.op("pe", [pg], [wg, xsT], fg, n=8)
                def fu(pu=pu, wg=wg, c=c, s0=s0, sn=sn):
                    for kc in range(8):
                        ins = nc.tensor.matmul(pu[:, 0:sn], wg[:, kc, D + c * 128:D + (c + 1) * 128], xsT[:, kc, s0:s0 + sn], start=(kc == 0), stop=(kc == 7))
                    return ins
                kb.op("pe", [pu], [wg, xsT], fu, n=8)
                kb.op("dve", [gp], [pg, bgu], lambda pg=pg, c=c, sn=sn, e=e: nc.vector.tensor_scalar(out=gp[:, 0:sn], in0=pg[:, 0:sn], scalar1=bgu[:, c, e:e + 1], scalar2=7.0, op0=ALU.add, op1=ALU.min))
                kb.op("act", [sgm], [gp], lambda sn=sn: nc.scalar.activation(out=sgm[:, 0:sn], in_=gp[:, 0:sn], func=AF.Sigmoid, scale=1.702))
                kb.op("dve", [up], [pu, bgu], lambda pu=pu, c=c, sn=sn, e=e: nc.vector.tensor_scalar(out=up[:, 0:sn], in0=pu[:, 0:sn], scalar1=bgu[:, 8 + c, e:e + 1], scalar2=-7.0, op0=ALU.add, op1=ALU.max))
                kb.op("dve", [up], [up], lambda sn=sn: nc.vector.tensor_scalar(out=up[:, 0:sn], in0=up[:, 0:sn], scalar1=7.0, scalar2=1.0, op0=ALU.min, op1=ALU.add))
                kb.op("dve", [gp], [gp, sgm], lambda sn=sn: nc.vector.tensor_tensor(out=gp[:, 0:sn], in0=gp[:, 0:sn], in1=sgm[:, 0:sn], op=ALU.mult))
                kb.op("dve", [hT], [gp, up], lambda c=c, s0=s0, sn=sn: nc.vector.tensor_tensor(out=hT[:, c, s0:s0 + sn], in0=gp[:, 0:sn], in1=up[:, 0:sn], op=ALU.mult))
        for st in range(CAP // 128):
            y_t = yst[yc % 2]
            yc += 1
            for half in range(2):
                pd = self.bank()
                def fd(pd=pd, wd=wd, st=st, half=half):
                    for c in range(8):
                        ins = nc.tensor.matmul(pd[:, :], hT[:, c, st * 128:(st + 1) * 128], wd[:, c, half * 512:(half + 1) * 512], start=(c == 0), stop=(c == 7))
                    return ins
                kb.op("pe", [pd], [hT, wd], fd, n=8)
                kb.op("dve", [y_t], [pd, bd], lambda pd=pd, half=half, y_t=y_t, bd=bd: nc.vector.tensor_tensor(out=y_t[:, half * 512:(half + 1) * 512], in0=pd[:, :], in1=bd[:, half * 512:(half + 1) * 512], op=ALU.add))
            kb.dma("sp", ys[e * CAP + st * 128:e * CAP + (st + 1) * 128, :], y_t[:], [], [y_t])
    kb.pop()
    kb.push()
    L = self.ln_setup(l, 2)
    gk = [kb.sb(f"mo_gk{i}", [128, D], F32) for i in range(4)]
    xin = [kb.sb(f"mo_cx{i}", [128, D], F32) for i in range(2)]
    acc = [kb.sb(f"mo_acc{i}", [128, D], F32) for i in range(2)]
    ob = [kb.sb(f"mo_co{i}", [128, D], F32) for i in range(2)]
    for i in range(NT):
        a_t, x_in, o_t = acc[i % 2], xin[i % 2], ob[i % 2]
        kb.dma("sp", x_in[:], xsrc[i * 128:(i + 1) * 128, :], [x_in], [])
        for k in range(4):
            kb.idma(gk[k][:, :], None, ys[:, :], bass.IndirectOffsetOnAxis(ap=didx[:, i, k:k + 1], axis=0), [gk[k]], [didx])
        kb.op("dve", [a_t], [x_in, gk[0], gts], lambda a_t=a_t, x_in=x_in, i=i: nc.vector.tensor_scalar(out=a_t[:], in0=gk[0][:], scalar1=gts[:, i, 0:1], scalar2=None, op0=ALU.mult))
        for k in range(1, 4):
            kb.op("dve", [a_t], [a_t, gk[k], gts], lambda a_t=a_t, k=k, i=i: nc.vector.scalar_tensor_tensor(out=a_t[:], in0=gk[k][:], scalar=gts[:, i, k:k + 1], in1=a_t[:], op0=ALU.mult, op1=ALU.add))
        kb.op("dve", [a_t], [a_t, x_in], lambda a_t=a_t, x_in=x_in: nc.vector.scalar_tensor_tensor(out=a_t[:], in0=x_in[:], scalar=ALPHA, in1=a_t[:], op0=ALU.mult, op1=ALU.add))
        self.ln_tile(L, a_t, o_t)
        kb.dma("sp", xdst[i * 128:(i + 1) * 128, :], o_t[:], [], [o_t])
    kb.pop()
    kb.pop()


Prog.p8_moe = _moe


def build_program(dbg=()):
    P = Prog(dbg=dbg)
    P.alloc_scratch()
    P.consts()
    P.bias_setup()
    xcur = P.din["x"]
    for l in range(DEPTH):
        P.p1_inproj(l, xcur)
        P.p23_nsa(l)
        P.p4_conv(l)
        P.p5_gla(l)
        P.p6_merge(l, xcur, P.scr["x1"])
        P.p7_xattn(l, P.scr["x1"], P.scr["x2"])
        last = (l == DEPTH - 1)
        P.p8_moe(l, P.scr["x2"], P.y if last else P.scr["x3"])
        xcur = P.scr["x3"]
    P.finish()
    return P


def make_in_maps(inputs, n=8):
    consts = {"c_ident": np.eye(128, dtype=np.float32), "c_tab": _const_tab(), "c_esel": _const_esel()}
    x = np.asarray(inputs["x"], np.float32)
    mem = np.asarray(inputs["mem"], np.float32)
    shared = {k: np.ascontiguousarray(np.asarray(inputs[k], np.float32)) for k in WNAMES}
    maps = []
    for b in range(n):
        m = {"x": np.ascontiguousarray(x[b]), "mem": np.ascontiguousarray(mem[b])}
        m.update(shared)
        m.update(consts)
        maps.append(m)
    return maps


def kernel(**inputs):
    P = build_program()
    maps = make_in_maps(inputs, 8)
    res = run_bass_kernel_spmd(P.nc, maps, core_ids=list(range(8)))
    out = np.stack([np.asarray(res.results[b]["y"], np.float32) for b in range(8)], axis=0)
    return out
```
